# Optimizing a Trainium2 kernel written in Bass

```python
import math
import jax
import jax.numpy as jnp
from jax import lax
import numpy as np

D_MODEL = 1024
BATCH = 4
SEQ = 8192
DEPTH = 1

MOBA_HEADS = 8
MOBA_HEAD_DIM = 64
MOBA_BLOCK = 256
MOBA_TOPK = 3
MOBA_Q_CHUNK = 64
DIFF_HEADS = 4
DIFF_HEAD_DIM = 64
ATTN_Q_BLOCK = 128
ROPE_THETA = 10000.0
N_GROUPS = 4
EXPERTS_PER_GROUP = 8
N_EXPERTS = N_GROUPS * EXPERTS_PER_GROUP
EXPERT_TOPK = 2
D_EXPERT = 512
NORM_EPS = 1e-6
NEG_INF = -1e30
MOBA_WIDTH = MOBA_HEADS * MOBA_HEAD_DIM
DIFF_QK_WIDTH = DIFF_HEADS * 2 * DIFF_HEAD_DIM
DIFF_V_WIDTH = DIFF_HEADS * 2 * DIFF_HEAD_DIM
_SEGMENTS = (MOBA_WIDTH, MOBA_WIDTH, MOBA_WIDTH, DIFF_QK_WIDTH, DIFF_QK_WIDTH, DIFF_V_WIDTH, D_MODEL, D_MODEL)
IN_WIDTH = sum(_SEGMENTS)
IN_SPLITS = tuple(int(s) for s in np.cumsum(_SEGMENTS)[:-1])

kernel_name = 'hybrid_moba_diffattn_hmoe_adaln'


def rmsnorm(x, g):
    xf = x.astype(jnp.float32)
    y = xf * lax.rsqrt(jnp.mean(xf * xf, axis=-1, keepdims=True) + NORM_EPS)
    return (y * g.astype(jnp.float32)).astype(x.dtype)


def rope_tables(seq, dim):
    inv = 1.0 / (ROPE_THETA ** (jnp.arange(0, dim, 2, dtype=jnp.float32) / dim))
    ang = jnp.arange(seq, dtype=jnp.float32)[:, None] * inv[None, :]
    return jnp.cos(ang), jnp.sin(ang)


def apply_rope(x, cos, sin):
    xf = x.astype(jnp.float32)
    half = x.shape[-1] // 2
    x1, x2 = xf[..., :half], xf[..., half:]
    return jnp.concatenate([x1 * cos - x2 * sin, x2 * cos + x1 * sin], axis=-1).astype(x.dtype)


def to_heads(t, n, d):
    b, s = t.shape[:2]
    return t.reshape(b, s, n, d).transpose(0, 2, 1, 3)


def moba_attention(q, k, v):
    B, H, S, dh = q.shape
    nb = -(-S // MOBA_BLOCK)
    topk = min(MOBA_TOPK, nb)
    pad = nb * MOBA_BLOCK - S
    kp = jnp.pad(k, ((0, 0), (0, 0), (0, pad), (0, 0)))
    vp = jnp.pad(v, ((0, 0), (0, 0), (0, pad), (0, 0)))
    k_blocks = kp.reshape(B, H, nb, MOBA_BLOCK, dh)
    v_blocks = vp.reshape(B, H, nb, MOBA_BLOCK, dh)
    k_mean = jnp.mean(k_blocks.astype(jnp.float32), axis=3)
    scale = dh ** -0.5
    nc = S // MOBA_Q_CHUNK
    q_chunks = q.reshape(B, H, nc, MOBA_Q_CHUNK, dh).transpose(2, 0, 1, 3, 4)
    starts = jnp.arange(nc, dtype=jnp.int32) * MOBA_Q_CHUNK
    b_idx = jnp.arange(B)[:, None, None, None]
    h_idx = jnp.arange(H)[None, :, None, None]
    block_ids = jnp.arange(nb, dtype=jnp.int32)
    own_offsets = jnp.arange(MOBA_BLOCK, dtype=jnp.int32)

    def chunk(args):
        qb, start = args
        blk = start // MOBA_BLOCK
        qpos = start + jnp.arange(MOBA_Q_CHUNK, dtype=jnp.int32)
        gate = jnp.einsum('bhqd,bhnd->bhqn', qb.astype(jnp.float32), k_mean)
        gate = jnp.where(block_ids < blk, gate, NEG_INF)
        _, sel = lax.top_k(gate, topk)
        valid = sel < blk
        ks = k_blocks[b_idx, h_idx, sel]
        vs = v_blocks[b_idx, h_idx, sel]
        s_sel = jnp.einsum('bhqd,bhqjnd->bhqjn', qb, ks).astype(jnp.float32) * scale
        s_sel = jnp.where(valid[..., None], s_sel, NEG_INF)
        s_sel = s_sel.reshape(B, H, MOBA_Q_CHUNK, topk * MOBA_BLOCK)
        k_own = lax.dynamic_slice_in_dim(kp, blk * MOBA_BLOCK, MOBA_BLOCK, axis=2)
        v_own = lax.dynamic_slice_in_dim(vp, blk * MOBA_BLOCK, MOBA_BLOCK, axis=2)
        s_own = jnp.einsum('bhqd,bhnd->bhqn', qb, k_own).astype(jnp.float32) * scale
        kpos = blk * MOBA_BLOCK + own_offsets
        s_own = jnp.where(kpos[None, :] <= qpos[:, None], s_own, NEG_INF)
        p = jax.nn.softmax(jnp.concatenate([s_sel, s_own], axis=-1), axis=-1).astype(v.dtype)
        p_sel = p[..., :topk * MOBA_BLOCK].reshape(B, H, MOBA_Q_CHUNK, topk, MOBA_BLOCK)
        p_own = p[..., topk * MOBA_BLOCK:]
        return (jnp.einsum('bhqjn,bhqjnd->bhqd', p_sel, vs)
                + jnp.einsum('bhqn,bhnd->bhqd', p_own, v_own))

    out = lax.map(chunk, (q_chunks, starts))
    return out.transpose(1, 2, 0, 3, 4).reshape(B, H, S, dh)


def diff_attention(q, k, v, lam):
    B, H2, S, dk = q.shape
    H = H2 // 2
    scale = dk ** -0.5
    nq = S // ATTN_Q_BLOCK
    q_blocks = q.reshape(B, H2, nq, ATTN_Q_BLOCK, dk).transpose(2, 0, 1, 3, 4)
    starts = jnp.arange(nq, dtype=jnp.int32) * ATTN_Q_BLOCK
    kpos = jnp.arange(S, dtype=jnp.int32)

    def block(args):
        qb, start = args
        s = jnp.einsum('bhqd,bhkd->bhqk', qb, k).astype(jnp.float32) * scale
        qpos = start + jnp.arange(ATTN_Q_BLOCK, dtype=jnp.int32)
        s = jnp.where(kpos[None, :] <= qpos[:, None], s, NEG_INF)
        p = jax.nn.softmax(s, axis=-1).reshape(B, H, 2, ATTN_Q_BLOCK, S)
        a = (p[:, :, 0] - lam * p[:, :, 1]).astype(v.dtype)
        return jnp.einsum('bhqk,bhkd->bhqd', a, v)

    out = lax.map(block, (q_blocks, starts))
    return out.transpose(1, 2, 0, 3, 4).reshape(B, H, S, 2 * dk)


def hierarchical_moe(h, w_group, b_group, w_expert, b_expert, w_gate, w_up, w_down):
    B, S, D = h.shape
    t = h.reshape(B * S, D)
    T = t.shape[0]
    g_prob = jax.nn.softmax((t @ w_group).astype(jnp.float32) + b_group, axis=-1)
    g_w, g_idx = lax.top_k(g_prob, 1)
    e_logits = ((t @ w_expert).astype(jnp.float32) + b_expert).reshape(T, N_GROUPS, EXPERTS_PER_GROUP)
    e_in_group = jnp.take_along_axis(e_logits, g_idx[:, :, None], axis=1)[:, 0]
    e_prob = jax.nn.softmax(e_in_group, axis=-1)
    e_w, e_idx = lax.top_k(e_prob, EXPERT_TOPK)
    e_w = e_w / jnp.sum(e_w, axis=-1, keepdims=True)
    weights = (g_w * e_w).reshape(-1)
    expert = (g_idx * EXPERTS_PER_GROUP + e_idx).reshape(-1)
    order = jnp.argsort(expert)
    tok = order // EXPERT_TOPK
    xs = t[tok]
    sizes = jnp.bincount(expert, length=N_EXPERTS).astype(jnp.int32)
    hid = jax.nn.silu(lax.ragged_dot(xs, w_gate, sizes)) * lax.ragged_dot(xs, w_up, sizes)
    out = lax.ragged_dot(hid.astype(xs.dtype), w_down, sizes)
    out = out * weights[order][:, None].astype(out.dtype)
    y = jnp.zeros_like(t).at[tok].add(out.astype(t.dtype))
    return y.reshape(B, S, D)


def setup_inputs(seed: int = 0) -> dict:
    key = jax.random.key(seed)
    ks = jax.random.split(key, 24)
    D = D_MODEL

    def nrm(k, shape, scale):
        return jax.random.normal(k, shape, jnp.float32) * scale

    return {
        'x': nrm(ks[0], (BATCH, SEQ, D), 1.0),
        'c': nrm(ks[1], (BATCH, D), 1.0),
        'w_ada': nrm(ks[2], (DEPTH, D, 6 * D), 0.5 * D ** -0.5),
        'b_ada': nrm(ks[3], (DEPTH, 6 * D), 0.02),
        'norm1_g': 1.0 + nrm(ks[4], (DEPTH, D), 0.02),
        'w_in': nrm(ks[5], (DEPTH, D, IN_WIDTH), D ** -0.5),
        'lambda_q1': nrm(ks[6], (DEPTH, DIFF_HEAD_DIM), 0.1),
        'lambda_k1': nrm(ks[7], (DEPTH, DIFF_HEAD_DIM), 0.1),
        'lambda_q2': nrm(ks[8], (DEPTH, DIFF_HEAD_DIM), 0.1),
        'lambda_k2': nrm(ks[9], (DEPTH, DIFF_HEAD_DIM), 0.1),
        'diff_subln_g': 1.0 + nrm(ks[10], (DEPTH, 2 * DIFF_HEAD_DIM), 0.02),
        'w_proj_moba': nrm(ks[11], (DEPTH, MOBA_WIDTH, D), MOBA_WIDTH ** -0.5),
        'w_proj_diff': nrm(ks[12], (DEPTH, DIFF_V_WIDTH, D), DIFF_V_WIDTH ** -0.5),
        'w_out': nrm(ks[13], (DEPTH, D, D), D ** -0.5),
        'norm2_g': 1.0 + nrm(ks[14], (DEPTH, D), 0.02),
        'w_group': nrm(ks[15], (DEPTH, D, N_GROUPS), D ** -0.5),
        'b_group': nrm(ks[16], (DEPTH, N_GROUPS), 0.01),
        'w_expert': nrm(ks[17], (DEPTH, D, N_EXPERTS), D ** -0.5),
        'b_expert': nrm(ks[18], (DEPTH, N_EXPERTS), 0.01),
        'w_gate': nrm(ks[19], (DEPTH, N_EXPERTS, D, D_EXPERT), D ** -0.5),
        'w_up': nrm(ks[20], (DEPTH, N_EXPERTS, D, D_EXPERT), D ** -0.5),
        'w_down': nrm(ks[21], (DEPTH, N_EXPERTS, D_EXPERT, D), D_EXPERT ** -0.5),
        'final_g': 1.0 + nrm(ks[22], (D,), 0.02),
    }


def reference(x, c, w_ada, b_ada, norm1_g, w_in, lambda_q1, lambda_k1, lambda_q2, lambda_k2,
              diff_subln_g, w_proj_moba, w_proj_diff, w_out, norm2_g, w_group, b_group,
              w_expert, b_expert, w_gate, w_up, w_down, final_g):
    S = x.shape[1]
    cos_m, sin_m = rope_tables(S, MOBA_HEAD_DIM)
    cos_d, sin_d = rope_tables(S, DIFF_HEAD_DIM)
    c_act = jax.nn.silu(c)
    for l in range(DEPTH):
        mod = c_act @ w_ada[l] + b_ada[l]
        sh1, sc1, g1, sh2, sc2, g2 = jnp.split(mod, 6, axis=-1)

        h = rmsnorm(x, norm1_g[l]) * (1.0 + sc1[:, None, :]) + sh1[:, None, :]
        proj = h @ w_in[l]
        q_m, k_m, v_m, q_d, k_d, v_d, gate_m, gate_d = jnp.split(proj, IN_SPLITS, axis=-1)

        qm = apply_rope(to_heads(q_m, MOBA_HEADS, MOBA_HEAD_DIM), cos_m, sin_m)
        km = apply_rope(to_heads(k_m, MOBA_HEADS, MOBA_HEAD_DIM), cos_m, sin_m)
        vm = to_heads(v_m, MOBA_HEADS, MOBA_HEAD_DIM)
        o_m = moba_attention(qm, km, vm)
        y_m = o_m.transpose(0, 2, 1, 3).reshape(x.shape[0], S, MOBA_WIDTH) @ w_proj_moba[l]

        lam_init = 0.8 - 0.6 * math.exp(-0.3 * l)
        lam = (jnp.exp(jnp.sum(lambda_q1[l].astype(jnp.float32) * lambda_k1[l].astype(jnp.float32)))
               - jnp.exp(jnp.sum(lambda_q2[l].astype(jnp.float32) * lambda_k2[l].astype(jnp.float32)))
               + lam_init)
        qd = apply_rope(to_heads(q_d, 2 * DIFF_HEADS, DIFF_HEAD_DIM), cos_d, sin_d)
        kd = apply_rope(to_heads(k_d, 2 * DIFF_HEADS, DIFF_HEAD_DIM), cos_d, sin_d)
        vd = to_heads(v_d, DIFF_HEADS, 2 * DIFF_HEAD_DIM)
        o_d = diff_attention(qd, kd, vd, lam)
        o_d = rmsnorm(o_d, diff_subln_g[l]) * (1.0 - lam_init)
        y_d = o_d.transpose(0, 2, 1, 3).reshape(x.shape[0], S, DIFF_V_WIDTH) @ w_proj_diff[l]

        merged = jax.nn.sigmoid(gate_m) * y_m + jax.nn.sigmoid(gate_d) * y_d
        x = x + g1[:, None, :] * (merged @ w_out[l])

        h2 = rmsnorm(x, norm2_g[l]) * (1.0 + sc2[:, None, :]) + sh2[:, None, :]
        y_ffn = hierarchical_moe(h2, w_group[l], b_group[l], w_expert[l], b_expert[l],
                                 w_gate[l], w_up[l], w_down[l])
        x = x + g2[:, None, :] * y_ffn
    return rmsnorm(x, final_g)
```

```python
import numpy as np
from contextlib import ExitStack
import concourse.bass as bass
import concourse.mybir as mybir
from concourse.bass_utils import run_bass_kernel_spmd

F32 = mybir.dt.float32
BF16 = mybir.dt.bfloat16
I32 = mybir.dt.int32
ALU = mybir.AluOpType
AF = mybir.ActivationFunctionType
AX = mybir.AxisListType

S = 8192
D = 1024
NO = 4096
NT_ALL = 64
NT_OWN = 32
CAP = 1024
EPS = 1e-6
OWN_CHUNKS = ([0, 3, 4, 7, 8, 11, 12, 15], [1, 2, 5, 6, 9, 10, 13, 14])


class Buf:
    __slots__ = ("name", "w", "r", "dsem", "dcnt")

    def __init__(self, name):
        self.name = name
        self.w = None
        self.r = {}
        self.dsem = None
        self.dcnt = 0


class Prog:
    ENGS = ("pe", "act", "dve", "pool", "sp")

    def __init__(self, nc):
        self.nc = nc
        self.ops = {e: [] for e in self.ENGS}
        self.cnt = {e: 0 for e in self.ENGS}
        self.seen = {e: {} for e in self.ENGS}
        self.sems = {}
        self._stack = []
        self.dmabufs = []
        for e in ("pe", "act", "dve", "pool"):
            self.sems[e] = self.new_sem("prog_" + e)

    def new_sem(self, name):
        self.nsem = getattr(self, "nsem", 0) + 1
        cm = self.nc.semaphore("%s_%d" % (name, self.nsem))
        s = cm.__enter__()
        self._stack.append(cm)
        return s

    def close(self):
        for cm in reversed(self._stack):
            cm.__exit__(None, None, None)

    def _need(self, eng, ev, waits):
        if ev is None:
            return
        sem, val, _ = ev
        k = id(sem)
        if self.seen[eng].get(k, 0) >= val:
            return
        if k in waits and waits[k][1] >= val:
            return
        waits[k] = (sem, val)

    def _flush(self, eng, waits):
        for k, (sem, val) in waits.items():
            self.seen[eng][k] = val
            self.ops[eng].append(("wait", sem, val))

    def _deps(self, eng, reads, writes, waw=True):
        waits = {}
        skip_same = (eng == "pe")
        for b in reads:
            if b.w is not None and not (skip_same and b.w[2] == eng):
                self._need(eng, b.w, waits)
        for b in writes:
            if waw and b.w is not None and not (skip_same and b.w[2] == eng):
                self._need(eng, b.w, waits)
            for ev in b.r.values():
                if ev[2] == eng:
                    continue
                self._need(eng, ev, waits)
        self._flush(eng, waits)

    def _commit(self, ev, reads, writes):
        k = id(ev[0])
        for b in reads:
            old = b.r.get(k)
            if old is None or old[1] < ev[1]:
                b.r[k] = ev
        for b in writes:
            b.w = ev
            b.r = {}

    def op(self, eng, fn, reads=(), writes=()):
        self._deps(eng, reads, writes)
        self.cnt[eng] += 1
        ev = (self.sems[eng], self.cnt[eng], eng)
        self.ops[eng].append(("inst", fn, self.sems[eng], 1))
        self._commit(ev, reads, writes)

    def dma(self, eng, fn, reads=(), writes=(), waw=True):
        sb = writes[0]
        if sb.dsem is None:
            sb.dsem = self.new_sem("d_" + sb.name)
            self.dmabufs.append(sb)
        self._deps(eng, reads, writes, waw=waw)
        sb.dcnt += 16
        ev = (sb.dsem, sb.dcnt, "dma")
        self.ops[eng].append(("inst", fn, sb.dsem, 16))
        self._commit(ev, reads, writes)

    def barrier(self):
        for e in self.ENGS:
            waits = {}
            for o in ("pe", "act", "dve", "pool"):
                if o != e and self.cnt[o] > 0:
                    self._need(e, (self.sems[o], self.cnt[o], o), waits)
            for b in self.dmabufs:
                self._need(e, (b.dsem, b.dcnt, "dma"), waits)
            self._flush(e, waits)

    def emit(self):
        nc = self.nc
        eng_map = {"sp": "sync", "pe": "tensor", "act": "scalar", "dve": "vector", "pool": "gpsimd"}
        with nc.Block() as block:
            def mk(e):
                def run(engobj):
                    for item in self.ops[e]:
                        if item[0] == "wait":
                            engobj.wait_ge(item[1], item[2])
                        else:
                            inst = item[1](engobj)
                            if item[2] is not None:
                                inst.then_inc(item[2], item[3])
                return run
            for e in self.ENGS:
                getattr(block, eng_map[e])(mk(e))
        self.ops = {e: [] for e in self.ENGS}


def MM(out, lhsT, rhs, start=True, stop=True):
    return lambda e: e.matmul(out, lhsT=lhsT, rhs=rhs, start=start, stop=stop)


def MMS(lst):
    def f(e):
        r = None
        for (out, lhsT, rhs, st, sp) in lst:
            r = e.matmul(out, lhsT=lhsT, rhs=rhs, start=st, stop=sp)
        return r
    return f


def TRS(lst):
    def f(e):
        r = None
        for (out, in_, ident) in lst:
            r = e.transpose(out=out, in_=in_, identity=ident)
        return r
    return f


def ACTF(out, in_, func, bias=0.0, scale=1.0, accum=None):
    if accum is None:
        return lambda e: e.activation(out=out, in_=in_, func=func, bias=bias, scale=scale)
    return lambda e: e.activation(out=out, in_=in_, func=func, bias=bias, scale=scale, accum_out=accum)


def CPY(out, in_):
    return lambda e: e.tensor_copy(out=out, in_=in_)


def ACPY(out, in_):
    return lambda e: e.copy(out=out, in_=in_)


def TT(out, in0, in1, op):
    return lambda e: e.tensor_tensor(out=out, in0=in0, in1=in1, op=op)


def TS(out, in0, s1, s2, op0, op1=None):
    if op1 is None:
        return lambda e: e.tensor_scalar(out=out, in0=in0, scalar1=s1, scalar2=None, op0=op0)
    return lambda e: e.tensor_scalar(out=out, in0=in0, scalar1=s1, scalar2=s2, op0=op0, op1=op1)


def STT(out, in0, scalar, in1, op0, op1):
    return lambda e: e.scalar_tensor_tensor(out=out, in0=in0, scalar=scalar, in1=in1, op0=op0, op1=op1)


def RED(out, in_, op, axis=AX.X):
    return lambda e: e.tensor_reduce(out=out, in_=in_, axis=axis, op=op)


def MSET(ap, v):
    return lambda e: e.memset(ap, v)


def DMA(out, in_):
    return lambda e: e.dma_start(out=out, in_=in_)


def bc(ap, shape):
    return ap.to_broadcast(shape)


def build(stage=99, cap=CAP, dbg=False):
    nc = bass.Bass("TRN2", target_bir_lowering=False)

    def din(name, shape, dt=F32):
        return nc.dram_tensor(name, list(shape), dt, kind="ExternalInput").ap()

    def dscr(name, shape, dt):
        kind = "ExternalOutput" if dbg else "Internal"
        return nc.dram_tensor(name, list(shape), dt, kind=kind).ap()

    xall = din("xall", [S, D])
    xown = din("xown", [NO, D])
    c_col = din("c_col", [128, 8])
    w_ada = din("w_ada", [D, 6 * D])
    bada_bc = din("bada_bc", [128, 6 * D])
    n1g_bc = din("n1g_bc", [128, D])
    n2g_bc = din("n2g_bc", [128, D])
    fing_bc = din("fing_bc", [128, D])
    w_in = din("w_in", [D, 5120])
    cs_all = din("cs_all", [128, NT_ALL, 64])
    cs_own = din("cs_own", [128, NT_OWN, 64])
    khot = din("khot", [33, S])
    cmask = din("cmask", [128, 16, 512])
    blkvalid = din("blkvalid", [128, NT_OWN, 32])
    ownblk = din("ownblk", [128, NT_OWN, 32])
    ident_h = din("ident", [128, 128])
    utri_h = din("utri", [128, 128])
    lam4 = din("lam4", [128, 4, 64])
    subg_bc = din("subg_bc", [128, 128])
    w_pm = din("w_pm", [512, D])
    w_pd = din("w_pd", [512, D])
    w_o = din("w_o", [D, D])
    w_r = din("w_r", [D, 36])
    br_bc = din("br_bc", [128, 36])
    ebase_bc = din("ebase_bc", [128, 32])
    w_gate = din("w_gate", [32, D, 512])
    w_up = din("w_up", [32, D, 512])
    w_down = din("w_down", [32, 512, D])
    out_h = nc.dram_tensor("out", [NO, D], F32, kind="ExternalOutput").ap()

    KT2 = dscr("KT2", [8, 128, S], BF16)
    VS = dscr("VS", [S, D], BF16)
    QT = dscr("QT", [128, 16, NO], BF16)
    HT = dscr("HT", [128, 8, NO], BF16)
    OM = dscr("OM", [NO, 512], BF16)
    OD = dscr("OD", [NO, 512], BF16)
    X1 = dscr("X1", [NO, D], F32)
    XS = dscr("XS", [32 * cap, D], BF16)
    YS = dscr("YS", [32 * cap, D], F32)
    BKT, BVS, BQT, BHT, BOM, BOD, BX1, BXS, BYS, BOUT = [Buf(n) for n in
        ("KT2", "VS", "QT", "HT", "OM", "OD", "X1", "XS", "YS", "OUT")]

    p = Prog(nc)
    top = ExitStack()

    uid = [0]
    regcache = {}

    def bnd(e, key):
        if key not in regcache:
            regcache[key] = e.to_reg(32 * cap - 1)
        return regcache[key]

    def sbt(es, name, shape, dt):
        uid[0] += 1
        return es.enter_context(nc.sbuf_tensor("%s_%d" % (name, uid[0]), list(shape), dt))

    def pst(es, name, shape, dt):
        uid[0] += 1
        return es.enter_context(nc.psum_tensor("%s_%d" % (name, uid[0]), list(shape), dt))

    ident_f = sbt(top, "ident_f", [128, 128], F32)
    ident_b = sbt(top, "ident_b", [128, 128], BF16)
    ones_f = sbt(top, "ones_f", [128, 128], F32)
    ones_b = sbt(top, "ones_b", [128, 128], BF16)
    utri_b = sbt(top, "utri_b", [128, 128], BF16)
    nhalf = sbt(top, "nhalf", [128, 4], F32)
    a1_col = sbt(top, "a1_col", [128, 8], F32)
    b1_col = sbt(top, "b1_col", [128, 8], F32)
    g1_bc = sbt(top, "g1_bc", [128, D], F32)
    a2_bc = sbt(top, "a2_bc", [128, D], F32)
    b2_bc = sbt(top, "b2_bc", [128, D], F32)
    g2_bc = sbt(top, "g2_bc", [128, D], F32)
    fing_sb = sbt(top, "fing_sb", [128, D], F32)
    kmT_sb = sbt(top, "kmT_sb", [128, 8, 32], F32)
    nkm_bc = sbt(top, "nkm_bc", [128, 16], F32)
    nlam = sbt(top, "nlam", [128, 1], F32)
    subg_sb = sbt(top, "subg_sb", [128, 128], F32)
    wr_sb = sbt(top, "wr_sb", [128, 8, 36], F32)
    br_sb = sbt(top, "br_sb", [128, 36], F32)
    ebase_sb = sbt(top, "ebase_sb", [128, 32], F32)
    pos_i = sbt(top, "pos_i", [128, NT_OWN, 2], I32)
    wts_sb = sbt(top, "wts_sb", [128, NT_OWN, 2], F32)
    carry = sbt(top, "carry", [128, 32], F32)

    with ExitStack() as es:
        c_sb = sbt(es, "c_sb", [128, 8], F32)
        cact = sbt(es, "cact", [128, 8], F32)
        cact_rep = sbt(es, "cact_rep", [128, 8, 128], F32)
        bada_sb = sbt(es, "bada_sb", [128, 6 * D], F32)
        mod_bc = sbt(es, "mod_bc", [128, 6 * D], F32)
        n1g_sb = sbt(es, "n1g_sb", [128, D], F32)
        n2g_sb = sbt(es, "n2g_sb", [128, D], F32)
        utri_f = sbt(es, "utri_f", [128, 128], F32)
        lam_sb = sbt(es, "lam_sb", [128, 4, 64], F32)
        lamt = sbt(es, "lamt", [128, 8], F32)
        tmpD = sbt(es, "tmpD", [128, D], F32)
        tmpE = sbt(es, "tmpE", [128, D], F32)
        zer = sbt(es, "zer", [128, 4, D], BF16)
        wa = [sbt(es, "wa%d" % i, [128, 8, 512], F32) for i in range(2)]
        pm = [pst(es, "pm%d" % i, [128, 512], F32) for i in range(2)]
        Bc, Bca, Bcr, Bbada, Bmod, Bn1, Bn2, Butf, Blam, Blt, BtD, BtE, Bzer = [Buf(n) for n in
            ("c", "cact", "cactrep", "bada", "mod", "n1g", "n2g", "utf", "lam", "lamt", "tmpD", "tmpE", "zer")]
        Bwa = [Buf("wa0"), Buf("wa1")]
        Bpm = [Buf("pm0"), Buf("pm1")]
        Bconst = Buf("const")
        Bpers = Buf("pers")

        for (dst, src) in ((ident_f[:], ident_h), (fing_sb[:], fing_bc), (subg_sb[:], subg_bc),
                           (br_sb[:], br_bc), (ebase_sb[:], ebase_bc),
                           (wr_sb[:], w_r.rearrange("(k p) n -> p k n", p=128))):
            p.dma("sp", DMA(dst, src), writes=[Bconst], waw=False)
        p.dma("sp", DMA(c_sb[:], c_col), writes=[Bc])
        p.dma("sp", DMA(bada_sb[:], bada_bc), writes=[Bbada])
        p.dma("sp", DMA(n1g_sb[:], n1g_bc), writes=[Bn1])
        p.dma("sp", DMA(n2g_sb[:], n2g_bc), writes=[Bn2])
        p.dma("sp", DMA(utri_f[:], utri_h), writes=[Butf])
        p.dma("sp", DMA(lam_sb[:], lam4), writes=[Blam])
        p.op("dve", MSET(ones_f[:], 1.0), writes=[Bpers])
        p.op("dve", MSET(ones_b[:], 1.0), writes=[Bpers])
        p.op("dve", MSET(nhalf[:], -0.5), writes=[Bpers])
        p.op("dve", MSET(carry[:], 0.0), writes=[Bpers])
        p.op("dve", MSET(zer[:], 0.0), writes=[Bzer])
        p.op("dve", CPY(ident_b[:], ident_f[:]), reads=[Bconst], writes=[Bpers])
        p.op("dve", CPY(utri_b[:], utri_f[:]), reads=[Butf], writes=[Bpers])
        p.op("dve", TS(subg_sb[:], subg_sb[:], 0.8, None, ALU.mult), reads=[Bconst], writes=[Bpers])
        p.op("dve", TT(lam_sb[:, 0, :], lam_sb[:, 0, :], lam_sb[:, 1, :], ALU.mult), reads=[Blam], writes=[Blam])
        p.op("dve", TT(lam_sb[:, 2, :], lam_sb[:, 2, :], lam_sb[:, 3, :], ALU.mult), reads=[Blam], writes=[Blam])
        p.op("dve", RED(lamt[:, 0:1], lam_sb[:, 0, :], ALU.add), reads=[Blam], writes=[Blt])
        p.op("dve", RED(lamt[:, 1:2], lam_sb[:, 2, :], ALU.add), reads=[Blam], writes=[Blt])
        p.op("act", ACTF(lamt[:, 2:4], lamt[:, 0:2], AF.Exp), reads=[Blt], writes=[Blt])
        p.op("dve", TT(lamt[:, 4:5], lamt[:, 3:4], lamt[:, 2:3], ALU.subtract), reads=[Blt], writes=[Blt])
        p.op("dve", TS(nlam[:], lamt[:, 4:5], -0.2, None, ALU.add), reads=[Blt], writes=[Bpers])
        p.op("act", ACTF(cact[:], c_sb[:], AF.Sigmoid), reads=[Bc], writes=[Bca])
        p.op("dve", TT(cact[:], cact[:], c_sb[:], ALU.mult), reads=[Bc, Bca], writes=[Bca])
        p.op("dve", CPY(cact_rep[:], bc(cact[:].unsqueeze(2), [128, 8, 128])), reads=[Bca], writes=[Bcr])
        for cg in range(12):
            sl = slice(cg * 512, (cg + 1) * 512)
            p.dma("sp", DMA(wa[cg % 2][:], w_ada[:, sl].rearrange("(k p) n -> p k n", p=128)), writes=[Bwa[cg % 2]])
            p.op("pe", MMS([(pm[cg % 2][:], cact_rep[:, k, :], wa[cg % 2][:, k, :], k == 0, k == 7) for k in range(8)]),
                 reads=[Bcr, Bwa[cg % 2]], writes=[Bpm[cg % 2]])
            p.op("dve", TT(mod_bc[:, sl], pm[cg % 2][:], bada_sb[:, sl], ALU.add),
                 reads=[Bpm[cg % 2], Bbada], writes=[Bmod])
        p.op("dve", STT(tmpD[:], mod_bc[:, D:2 * D], 1.0, n1g_sb[:], ALU.add, ALU.mult), reads=[Bmod, Bn1], writes=[BtD])
        p.op("dve", TT(tmpE[:].rearrange("p (k j) -> p k j", j=128), tmpD[:].rearrange("p (k j) -> p k j", j=128),
                       bc(ident_f[:].unsqueeze(1), [128, 8, 128]), ALU.mult), reads=[BtD, Bconst], writes=[BtE])
        p.op("dve", RED(a1_col[:], tmpE[:].rearrange("p (k j) -> p k j", j=128), ALU.add), reads=[BtE], writes=[Bpers])
        p.op("dve", TT(tmpE[:].rearrange("p (k j) -> p k j", j=128), mod_bc[:, 0:D].rearrange("p (k j) -> p k j", j=128),
                       bc(ident_f[:].unsqueeze(1), [128, 8, 128]), ALU.mult), reads=[Bmod, Bconst], writes=[BtE])
        p.op("dve", RED(b1_col[:], tmpE[:].rearrange("p (k j) -> p k j", j=128), ALU.add), reads=[BtE], writes=[Bpers])
        p.op("dve", CPY(g1_bc[:], mod_bc[:, 2 * D:3 * D]), reads=[Bmod], writes=[Bpers])
        p.op("dve", CPY(b2_bc[:], mod_bc[:, 3 * D:4 * D]), reads=[Bmod], writes=[Bpers])
        p.op("dve", STT(a2_bc[:], mod_bc[:, 4 * D:5 * D], 1.0, n2g_sb[:], ALU.add, ALU.mult), reads=[Bmod, Bn2], writes=[Bpers])
        p.op("dve", CPY(g2_bc[:], mod_bc[:, 5 * D:6 * D]), reads=[Bmod], writes=[Bpers])
        p.barrier()
        p.emit()

    if stage < 1:
        return finish(nc, p, top, out_h, BOUT)

    def norm_part(x_t, Bx, ss, Bss, sqj, Bsqj, xn, Bxn):
        p.op("act", ACTF(sqj[:], x_t[:], AF.Square, accum=ss[:, 0:1]), reads=[Bx], writes=[Bsqj, Bss])
        p.op("dve", TS(ss[:, 1:2], ss[:, 0:1], 1.0 / D, EPS, ALU.mult, ALU.add), reads=[Bss], writes=[Bss])
        p.op("pool", TT(ss[:, 2:3], ss[:, 1:2], nhalf[:, 0:1], ALU.pow), reads=[Bss], writes=[Bss])
        p.op("dve", TS(xn[:], x_t[:], ss[:, 2:3], None, ALU.mult), reads=[Bx, Bss], writes=[Bxn])

    def transpose_pe(xn, Bxn, psT, BpsT):
        p.op("pe", TRS([(psT[:, k * 128:(k + 1) * 128], xn[:, k * 128:(k + 1) * 128], ident_b[:]) for k in range(8)]),
             reads=[Bxn], writes=[BpsT])

    def transpose_evac(psT, BpsT, hT, BhT):
        for k in range(8):
            if k % 4 == 0:
                p.op("dve", TS(hT[:, k, :], psT[:, k * 128:(k + 1) * 128], a1_col[:, k:k + 1], b1_col[:, k:k + 1],
                               ALU.mult, ALU.add), reads=[BpsT], writes=[BhT])
            else:
                p.op("act", ACTF(hT[:, k, :], psT[:, k * 128:(k + 1) * 128], AF.Identity,
                                 bias=b1_col[:, k:k + 1], scale=a1_col[:, k:k + 1]), reads=[BpsT], writes=[BhT])

    def pipeline(nt, stages):
        ns = len(stages)
        for s_ in range(nt + ns - 1):
            for k_ in range(ns - 1, -1, -1):
                t_ = s_ - k_
                if 0 <= t_ < nt:
                    stages[k_](t_)

    def rope(src, dst, cs_t, Bsrc, Bdst, t1, t2, Bt1, Bt2, nh):
        sv = src.rearrange("p (h two d) -> p h two d", two=2, d=32)
        dv = dst.rearrange("p (h two d) -> p h two d", two=2, d=32)
        x1, x2 = sv[:, :, 0, :], sv[:, :, 1, :]
        cosb = bc(cs_t[:, 0:32].unsqueeze(1), [128, nh, 32])
        sinb = bc(cs_t[:, 32:64].unsqueeze(1), [128, nh, 32])
        a = t1.rearrange("p (h d) -> p h d", d=32)
        b = t2.rearrange("p (h d) -> p h d", d=32)
        p.op("dve", TT(a, x1, cosb, ALU.mult), reads=[Bsrc], writes=[Bt1])
        p.op("dve", TT(b, x2, sinb, ALU.mult), reads=[Bsrc], writes=[Bt2])
        p.op("dve", TT(dv[:, :, 0, :], a, b, ALU.subtract), reads=[Bt1, Bt2], writes=[Bdst])
        p.op("dve", TT(a, x2, cosb, ALU.mult), reads=[Bsrc, Bdst], writes=[Bt1])
        p.op("dve", TT(b, x1, sinb, ALU.mult), reads=[Bsrc, Bdst], writes=[Bt2])
        p.op("dve", TT(dv[:, :, 1, :], a, b, ALU.add), reads=[Bt1, Bt2], writes=[Bdst])

    with ExitStack() as es:
        wkv = sbt(es, "wkv", [128, 8, 2048], BF16)
        csA = sbt(es, "csA", [128, NT_ALL, 64], F32)
        xt = [sbt(es, "xt%d" % i, [128, D], F32) for i in range(3)]
        ss = [sbt(es, "ss%d" % i, [128, 4], F32) for i in range(2)]
        sqj = sbt(es, "sqj", [128, D], BF16)
        xn = [sbt(es, "xn%d" % i, [128, D], BF16) for i in range(2)]
        hT = [sbt(es, "hT%d" % i, [128, 8, 128], BF16) for i in range(2)]
        kraw = [sbt(es, "kraw%d" % i, [128, D], F32) for i in range(2)]
        kr = [sbt(es, "kr%d" % i, [128, D], F32) for i in range(2)]
        t1 = sbt(es, "t1", [128, 512], F32)
        t2 = sbt(es, "t2", [128, 512], F32)
        sq = sbt(es, "sq", [128, D], F32)
        kss = sbt(es, "kss", [128, 16], F32)
        kmaxacc = sbt(es, "kmaxacc", [128, 16], F32)
        kb = [sbt(es, "kb%d" % i, [128, D], BF16) for i in range(2)]
        vst = [sbt(es, "vst%d" % i, [128, D], BF16) for i in range(2)]
        ktst = [sbt(es, "ktst%d" % i, [128, 8, 512], BF16) for i in range(2)]
        kmx = sbt(es, "kmx", [16, 4], F32)
        dg = sbt(es, "dg", [16, 16], F32)
        psT = pst(es, "psT", [128, D], BF16)
        pp = pst(es, "pp", [128, 2048], F32)
        pkt = pst(es, "pkt", [128, D], BF16)
        pkm = pst(es, "pkm", [128, 128], F32)
        pmisc = pst(es, "pmisc", [128, 128], F32)
        Bwkv, BcsA, Bsqj, Bt1, Bt2, Bsq, Bkss, Bkma, BpsT, Bpp, Bpkt, Bpkm, Bpmisc, Bkmx, Bdg = [Buf(n) for n in
            ("wkv", "csA", "sqj", "t1", "t2", "sq", "kss", "kma", "psT", "pp", "pkt", "pkm", "pmisc", "kmx", "dg")]
        Bxt = [Buf("xt%d" % i) for i in range(3)]
        Bss = [Buf("ss%d" % i) for i in range(2)]
        Bxn = [Buf("xn%d" % i) for i in range(2)]
        BhT = [Buf("hT%d" % i) for i in range(2)]
        Bkraw = [Buf("kraw%d" % i) for i in range(2)]
        Bkr = [Buf("kr%d" % i) for i in range(2)]
        Bkb = [Buf("kb%d" % i) for i in range(2)]
        Bvst = [Buf("vst%d" % i) for i in range(2)]
        Bktst = [Buf("ktst%d" % i) for i in range(2)]

        segs = [(512, 1024), (1024, 1536), (2048, 2560), (2560, 3072)]
        for gi, (a, b) in enumerate(segs):
            p.dma("pool", DMA(wkv[:, :, gi * 512:(gi + 1) * 512], w_in[:, a:b].rearrange("(k p) n -> p k n", p=128)),
                  writes=[Bwkv], waw=False)
        p.dma("sp", DMA(csA[:], cs_all), writes=[BcsA])
        p.op("dve", MSET(kmaxacc[:], 0.0), writes=[Bkma])
        Bppa, Bppb = Buf("ppa"), Buf("ppb")

        def a0(t):
            p.dma("sp", DMA(xt[t % 3][:], xall[t * 128:(t + 1) * 128, :]), writes=[Bxt[t % 3]])

        def a1(t):
            u = t % 2
            norm_part(xt[t % 3], Bxt[t % 3], ss[u], Bss[u], sqj, Bsqj, xn[u], Bxn[u])

        def a2a(t):
            u = t % 2
            transpose_pe(xn[u], Bxn[u], psT, BpsT)

        def a2b(t):
            u = t % 2
            transpose_evac(psT, BpsT, hT[u], BhT[u])

        def b1(t):
            u = t % 2
            p.op("pe", MMS([(pp[:, g * 512:(g + 1) * 512], hT[u][:, k, :], wkv[:, k, g * 512:(g + 1) * 512], k == 0, k == 7)
                            for k in range(8) for g in (0, 2)]), reads=[BhT[u], Bwkv], writes=[Bppa])
            p.op("pe", MMS([(pp[:, g * 512:(g + 1) * 512], hT[u][:, k, :], wkv[:, k, g * 512:(g + 1) * 512], k == 0, k == 7)
                            for k in range(8) for g in (1, 3)]), reads=[BhT[u], Bwkv], writes=[Bppb])

        def b2(t):
            u = t % 2
            p.op("act", ACPY(kraw[u][:, 0:512], pp[:, 0:512]), reads=[Bppa], writes=[Bkraw[u]])
            p.op("act", ACPY(kraw[u][:, 512:1024], pp[:, 1024:1536]), reads=[Bppa], writes=[Bkraw[u]])
            p.op("act", ACPY(vst[u][:, 0:512], pp[:, 512:1024]), reads=[Bppb], writes=[Bvst[u]])
            p.op("dve", CPY(vst[u][:, 512:1024], pp[:, 1536:2048]), reads=[Bppb], writes=[Bvst[u]])
            p.dma("act", DMA(VS[t * 128:(t + 1) * 128, :], vst[u][:]), reads=[Bvst[u]], writes=[BVS], waw=False)

        def c1(t):
            u = t % 2
            rope(kraw[u][:], kr[u][:], csA[:, t, :], Bkraw[u], Bkr[u], t1[:], t2[:], Bt1, Bt2, 16)
            p.op("pool", TT(sq[:], kr[u][:], kr[u][:], ALU.mult), reads=[Bkr[u]], writes=[Bsq])
            p.op("dve", RED(kss[:], sq[:].rearrange("p (h d) -> p h d", d=64), ALU.add), reads=[Bsq], writes=[Bkss])
            p.op("dve", TT(kmaxacc[:], kmaxacc[:], kss[:], ALU.max), reads=[Bkss], writes=[Bkma])
            p.op("act", ACPY(kb[u][:], kr[u][:]), reads=[Bkr[u]], writes=[Bkb[u]])

        def c2(t):
            u = t % 2
            n = t // 2
            p.op("pe", MMS([(pkm[:, j * 32 + n:j * 32 + n + 1], kr[u][:, j * 128:(j + 1) * 128], ones_f[:, 0:1],
                             t % 2 == 0 and j == 0, t % 2 == 1) for j in range(4)]), reads=[Bkr[u]], writes=[Bpkm])
            p.op("pe", TRS([(pkt[:, j * 128:(j + 1) * 128], kb[u][:, j * 128:(j + 1) * 128], ident_b[:]) for j in range(8)]),
                 reads=[Bkb[u]], writes=[Bpkt])

        def c3(t):
            g, tt = t // 4, t % 4
            p.op("dve", CPY(ktst[g % 2][:, :, tt * 128:(tt + 1) * 128], pkt[:].rearrange("p (j n) -> p j n", n=128)),
                 reads=[Bpkt], writes=[Bktst[g % 2]])
            if tt == 3:
                p.dma("act", DMA(KT2[:, :, g * 512:(g + 1) * 512].rearrange("j p s -> p j s"), ktst[g % 2][:]),
                      reads=[Bktst[g % 2]], writes=[BKT], waw=False)

        pipeline(NT_ALL, [a0, a1, a2a, a2b, b1, b2, c1, c2, c3])
        p.op("dve", MSET(kmT_sb[:], 0.0), writes=[Bpers])
        pkv = pkm[:].rearrange("p (j n) -> p j n", n=32)
        kmv = kmT_sb[:].rearrange("p (j two) n -> p j two n", two=2)
        p.op("dve", CPY(kmv[0:64, :, 0, :], pkv[0:64, :, :]), reads=[Bpkm], writes=[Bpers])
        p.op("dve", CPY(kmv[64:128, :, 1, :], pkv[64:128, :, :]), reads=[Bpkm], writes=[Bpers])
        p.op("pe", TRS([(pmisc[0:16, 0:128], kmaxacc[:, 0:16], ident_f[:])]), reads=[Bkma], writes=[Bpmisc])
        p.op("dve", RED(kmx[:, 0:1], pmisc[0:16, 0:128], ALU.max), reads=[Bpmisc], writes=[Bkmx])
        p.op("dve", TS(dg[:], ident_f[0:16, 0:16], kmx[:, 0:1], -0.0625, ALU.mult, ALU.mult), reads=[Bkmx], writes=[Bdg])
        p.op("pe", MM(pmisc[:, 0:16], ones_f[0:16, 0:128], dg[:]), reads=[Bdg, Bkmx], writes=[Bpmisc])
        p.op("dve", CPY(nkm_bc[:], pmisc[:, 0:16]), reads=[Bpmisc], writes=[Bpers])
        p.barrier()
        p.emit()

    if stage < 2:
        return finish(nc, p, top, out_h, BOUT)

    with ExitStack() as es:
        wq = sbt(es, "wq", [128, 8, 1024], BF16)
        csO = sbt(es, "csO", [128, NT_OWN, 64], F32)
        bval = sbt(es, "bval", [128, NT_OWN, 32], F32)
        oblk = sbt(es, "oblk", [128, NT_OWN, 32], F32)
        xt = [sbt(es, "xt%d" % i, [128, D], F32) for i in range(3)]
        ss = [sbt(es, "ss%d" % i, [128, 4], F32) for i in range(2)]
        sqj = sbt(es, "sqj", [128, D], BF16)
        xn = [sbt(es, "xn%d" % i, [128, D], BF16) for i in range(2)]
        hst = [sbt(es, "hst%d" % i, [128, 8, 512], BF16) for i in range(2)]
        qraw = [sbt(es, "qraw%d" % i, [128, D], F32) for i in range(2)]
        qr = [sbt(es, "qr%d" % i, [128, D], F32) for i in range(2)]
        t1 = sbt(es, "t1", [128, 512], F32)
        t2 = sbt(es, "t2", [128, 512], F32)
        sq = sbt(es, "sq", [128, D], F32)
        qss = sbt(es, "qss", [128, 16], F32)
        A = [sbt(es, "A%d" % i, [128, 16, 128], BF16) for i in range(4)]
        qT = sbt(es, "qT", [128, 4, 128], F32)
        gm2 = [sbt(es, "gm%d" % i, [128, 8, 32], F32) for i in range(2)]
        Bgm2 = [Buf("gm0"), Buf("gm1")]
        top8 = sbt(es, "top8", [128, 8, 8], F32)
        sel = sbt(es, "sel", [128, 8, 32], F32)
        val = sbt(es, "val", [128, 8, 32], F32)
        qtst = [sbt(es, "qtst%d" % i, [128, 16, 512], BF16) for i in range(2)]
        psT = pst(es, "psT", [128, D], BF16)
        pq = pst(es, "pq", [128, D], F32)
        pqT = pst(es, "pqT", [128, 512], F32)
        pg = pst(es, "pg", [128, 256], F32)
        pA = pst(es, "pA", [128, 2048], BF16)
        Bwq, BcsO, Bbv, Bob, Bsqj, Bt1, Bt2, Bsq, Bqss, BqT, Bgm, Btop8, Bsel, Bval, BpsT, Bpq, BpqT, Bpg, BpA = [
            Buf(n) for n in ("wq", "csO", "bv", "ob", "sqj", "t1", "t2", "sq", "qss", "qT", "gm", "top8", "sel", "val",
                             "psT", "pq", "pqT", "pg", "pA")]
        Bxt = [Buf("xt%d" % i) for i in range(3)]
        Bss = [Buf("ss%d" % i) for i in range(2)]
        Bxn = [Buf("xn%d" % i) for i in range(2)]
        Bhst = [Buf("hst%d" % i) for i in range(2)]
        Bqraw = [Buf("qraw%d" % i) for i in range(2)]
        Bqr = [Buf("qr%d" % i) for i in range(2)]
        BA = [Buf("A%d" % i) for i in range(4)]
        Bqtst = [Buf("qtst%d" % i) for i in range(2)]

        for gi, (a, b) in enumerate([(0, 512), (1536, 2048)]):
            p.dma("pool", DMA(wq[:, :, gi * 512:(gi + 1) * 512], w_in[:, a:b].rearrange("(k p) n -> p k n", p=128)),
                  writes=[Bwq], waw=False)
        p.dma("sp", DMA(csO[:], cs_own), writes=[BcsO])
        p.dma("sp", DMA(bval[:], blkvalid), writes=[Bbv])
        p.dma("sp", DMA(oblk[:], ownblk), writes=[Bob])
        for i in range(4):
            p.op("dve", MSET(A[i][:], 0.0), writes=[BA[i]])
        def a0(t):
            p.dma("sp", DMA(xt[t % 3][:], xown[t * 128:(t + 1) * 128, :]), writes=[Bxt[t % 3]])

        def a1(t):
            u = t % 2
            norm_part(xt[t % 3], Bxt[t % 3], ss[u], Bss[u], sqj, Bsqj, xn[u], Bxn[u])

        class _H:
            def __init__(self, v):
                self.v = v

            def __getitem__(self, key):
                return self.v[key]

        def a2a(t):
            u = t % 2
            transpose_pe(xn[u], Bxn[u], psT, BpsT)

        def a2b(t):
            g, tt = t // 4, t % 4
            hTv = hst[g % 2][:, :, tt * 128:(tt + 1) * 128]
            transpose_evac(psT, BpsT, _H(hTv), Bhst[g % 2])

        def b1(t):
            g, tt = t // 4, t % 4
            hTv = hst[g % 2][:, :, tt * 128:(tt + 1) * 128]
            p.op("pe", MMS([(pq[:, gq * 512:(gq + 1) * 512], hTv[:, k, :], wq[:, k, gq * 512:(gq + 1) * 512], k == 0, k == 7)
                            for k in range(8) for gq in range(2)]), reads=[Bhst[g % 2], Bwq], writes=[Bpq])
            if tt == 3:
                p.dma("act", DMA(HT[:, :, g * 512:(g + 1) * 512], hst[g % 2][:]), reads=[Bhst[g % 2]], writes=[BHT], waw=False)

        def b2(t):
            u = t % 2
            p.op("act", ACPY(qraw[u][:, 0:512], pq[:, 0:512]), reads=[Bpq], writes=[Bqraw[u]])
            p.op("act", ACPY(qraw[u][:, 512:1024], pq[:, 512:1024]), reads=[Bpq], writes=[Bqraw[u]])

        def c1(t):
            u = t % 2
            rope(qraw[u][:], qr[u][:], csO[:, t, :], Bqraw[u], Bqr[u], t1[:], t2[:], Bt1, Bt2, 16)
            p.op("pool", TT(sq[:], qr[u][:], qr[u][:], ALU.mult), reads=[Bqr[u]], writes=[Bsq])
            p.op("dve", RED(qss[:], sq[:].rearrange("p (h d) -> p h d", d=64), ALU.add), reads=[Bsq], writes=[Bqss])
            Au, BAu = A[t % 4], BA[t % 4]
            p.op("dve", STT(Au[:, :, 96], qss[:], -0.0625, nkm_bc[:], ALU.mult, ALU.add), reads=[Bqss], writes=[BAu])
            p.op("act", ACTF(Au[:, :, 0:64], qr[u][:].rearrange("p (h d) -> p h d", d=64), AF.Copy, scale=0.125),
                 reads=[Bqr[u]], writes=[BAu])

        def c1b(t):
            u = t % 2
            p.op("pe", TRS([(pqT[:, j * 128:(j + 1) * 128], qr[u][:, j * 128:(j + 1) * 128], ident_f[:]) for j in range(4)]),
                 reads=[Bqr[u]], writes=[BpqT])
            p.op("act", ACPY(qT[:], pqT[:].rearrange("p (j n) -> p j n", n=128)), reads=[BpqT], writes=[BqT])
            p.op("pe", MMS([(pg[:, h * 32:(h + 1) * 32], qT[:, h // 2, :], kmT_sb[:, h, :], True, True) for h in range(8)]),
                 reads=[BqT], writes=[Bpg])
            p.op("dve", TT(gm2[u][:], pg[:].rearrange("p (h n) -> p h n", n=32), bc(bval[:, t, :].unsqueeze(1), [128, 8, 32]),
                           ALU.add), reads=[Bpg, Bbv], writes=[Bgm2[u]])

        def c2(t):
            u = t % 2
            Au, BAu = A[t % 4], BA[t % 4]
            gm, Bgm = gm2[u], Bgm2[u]
            for h in range(8):
                p.op("dve", (lambda o, i: (lambda e: e.max(out=o, in_=i)))(top8[:, h, :], gm[:, h, :]),
                     reads=[Bgm], writes=[Btop8])
            p.op("dve", TT(sel[:], gm[:], bc(top8[:, :, 2:3], [128, 8, 32]), ALU.is_ge), reads=[Bgm, Btop8], writes=[Bsel])
            p.op("dve", TS(val[:], gm[:], -1e29, None, ALU.is_gt), reads=[Bgm], writes=[Bval])
            p.op("dve", TT(sel[:], sel[:], val[:], ALU.mult), reads=[Bval], writes=[Bsel])
            p.op("dve", TT(sel[:], sel[:], bc(oblk[:, t, :].unsqueeze(1), [128, 8, 32]), ALU.max), reads=[Bob], writes=[Bsel])
            p.op("dve", TS(Au[:, 0:8, 64:96], sel[:], 30000.0, -30000.0, ALU.mult, ALU.add), reads=[Bsel], writes=[BAu])

        def c3(t):
            g, tt = t // 4, t % 4
            Au, BAu = A[t % 4], BA[t % 4]
            p.op("pe", TRS([(pA[:, h * 128:(h + 1) * 128], Au[:, h, :], ident_b[:]) for h in range(16)]),
                 reads=[BAu], writes=[BpA])

        def c4(t):
            g, tt = t // 4, t % 4
            pAv = pA[:].rearrange("p (h n) -> p h n", n=128)
            p.op("dve", CPY(qtst[g % 2][:, 0:8, tt * 128:(tt + 1) * 128], pAv[:, 0:8, :]), reads=[BpA], writes=[Bqtst[g % 2]])
            p.op("act", ACPY(qtst[g % 2][:, 8:16, tt * 128:(tt + 1) * 128], pAv[:, 8:16, :]), reads=[BpA], writes=[Bqtst[g % 2]])
            if tt == 3:
                p.dma("act", DMA(QT[:, :, g * 512:(g + 1) * 512], qtst[g % 2][:]), reads=[Bqtst[g % 2]], writes=[BQT], waw=False)

        pipeline(NT_OWN, [a0, a1, a2a, a2b, b1, b2, c1, c1b, c2, c3, c4])
        p.barrier()
        p.emit()

    if stage < 3:
        return finish(nc, p, top, out_h, BOUT)

    with ExitStack() as es:
        KTs = [sbt(es, "KTs%d" % i, [128, S], BF16) for i in range(2)]
        QTs = [sbt(es, "QTs%d" % i, [128, NO], BF16) for i in range(2)]
        Vm = [sbt(es, "Vm%d" % i, [128, NT_ALL, 65], BF16) for i in range(2)]
        Vd = [sbt(es, "Vd%d" % i, [128, NT_ALL, 129], BF16) for i in range(2)]
        cm = sbt(es, "cm", [128, 16, 512], BF16)
        PT = [sbt(es, "PT%d" % i, [128, 1024], BF16) for i in range(3)]
        o1s = sbt(es, "o1s", [128, NT_OWN, 128], F32)
        rinv = sbt(es, "rinv", [128, 4], F32)
        omst = [sbt(es, "omst%d" % i, [128, 4, 64], BF16) for i in range(2)]
        odst = [sbt(es, "odst%d" % i, [128, 4, 128], BF16) for i in range(2)]
        o2t = sbt(es, "o2t", [128, 4, 128], F32)
        odt = sbt(es, "odt", [128, 4, 128], F32)
        sq4 = sbt(es, "sq4", [128, 4, 128], F32)
        st4 = sbt(es, "st4", [128, 8], F32)
        pS = [pst(es, "pS%d" % i, [128, 1024], F32) for i in range(2)]
        pO = [pst(es, "pO%d" % i, [128, 1024], F32) for i in range(2)]
        BKTs = [Buf("KTs%d" % i) for i in range(2)]
        BQTs = [Buf("QTs%d" % i) for i in range(2)]
        BVm = [Buf("Vm%d" % i) for i in range(2)]
        BVd = [Buf("Vd%d" % i) for i in range(2)]
        BPT = [Buf("PT%d" % i) for i in range(3)]
        BpS = [Buf("pS%d" % i) for i in range(2)]
        BpO = [Buf("pO%d" % i) for i in range(2)]
        Bcm, Bo1s, Brinv, Bo2t, Bodt, Bsq4, Bst4 = [Buf(n) for n in ("cm", "o1s", "rinv", "o2t", "odt", "sq4", "st4")]
        Bomst = [Buf("omst%d" % i) for i in range(2)]
        Bodst = [Buf("odst%d" % i) for i in range(2)]

        p.dma("pool", DMA(cm[:], cmask), writes=[Bcm])
        for i in range(2):
            p.dma("pool", DMA(KTs[i][64:97, :], khot), writes=[BKTs[i]])
            p.op("dve", MSET(Vm[i][:, :, 64:65], 1.0), writes=[BVm[i]])
            p.op("dve", MSET(Vd[i][:, :, 128:129], 1.0), writes=[BVd[i]])
        VSv = VS.rearrange("(t p) c -> p t c", p=128)
        OMv = OM.rearrange("(n p) c -> p n c", p=128)
        ODv = OD.rearrange("(n p) c -> p n c", p=128)

        def load_head(hi):
            u = hi % 2
            p.dma("sp", DMA(KTs[u][0:64, :], KT2[hi // 2, (hi % 2) * 64:(hi % 2) * 64 + 64, :]), reads=[BKT], writes=[BKTs[u]])
            p.dma("sp", DMA(QTs[u][:], QT[:, hi, :]), reads=[BQT], writes=[BQTs[u]])
            if hi < 8:
                p.dma("sp", DMA(Vm[u][:, :, 0:64], VSv[:, :, hi * 64:(hi + 1) * 64]), reads=[BVS], writes=[BVm[u]])
            elif (hi - 8) % 2 == 0:
                hd = (hi - 8) // 2
                p.dma("sp", DMA(Vd[hd % 2][:, :, 0:128], VSv[:, :, 512 + hd * 128:512 + (hd + 1) * 128]),
                      reads=[BVS], writes=[BVd[hd % 2]])

        head_list = list(range(16))
        items = []
        for hn, hi in enumerate(head_list):
            for i in range(8):
                for pr in range(4 * (i + 1)):
                    items.append((hn, hi, i, pr))
        N = len(items)
        oidx_of = {}
        for (hn, hi, i, pr) in items:
            if (hn, i) not in oidx_of:
                oidx_of[(hn, i)] = len(oidx_of)

        def emit_qk(n):
            hn, hi, i, pr = items[n]
            u = hi % 2
            ps_, Bps = pS[n % 2], BpS[n % 2]
            kt0 = 2 * pr
            p.op("pe", MMS([(ps_[:, kk * 512:(kk + 1) * 512], KTs[u][0:97, (kt0 + kk) * 128:(kt0 + kk + 1) * 128],
                             QTs[u][0:97, i * 512:(i + 1) * 512], True, True) for kk in range(2)]),
                 reads=[BKTs[u], BQTs[u]], writes=[Bps])

        def emit_exp(n):
            hn, hi, i, pr = items[n]
            ps_, Bps = pS[n % 2], BpS[n % 2]
            pt, Bpt = PT[n % 3], BPT[n % 3]
            nkt = 8 * (i + 1)
            kt0 = 2 * pr
            p.op("act", ACTF(pt[:], ps_[:], AF.Exp), reads=[Bps], writes=[Bpt])
            if kt0 >= nkt - 8:
                m0 = (i % 2) * 8 + (kt0 - (nkt - 8))
                p.op("dve", TT(pt[:], pt[:], cm[:, m0:m0 + 2, :].rearrange("p a q -> p (a q)"), ALU.mult),
                     reads=[Bcm], writes=[Bpt])

        def emit_av(n):
            hn, hi, i, pr = items[n]
            u = hi % 2
            moba = hi < 8
            if i == 0 and pr == 0 and hn + 1 < len(head_list):
                load_head(head_list[hn + 1])
            if moba:
                Vt, BV, W = Vm[u], BVm[u], 65
            else:
                hd, cc = (hi - 8) // 2, (hi - 8) % 2
                Vt, BV, W = Vd[hd % 2], BVd[hd % 2], 129
            npairs = 4 * (i + 1)
            oi = oidx_of[(hn, i)]
            po, Bpo = pO[oi % 2], BpO[oi % 2]
            pt, Bpt = PT[n % 3], BPT[n % 3]
            kt0 = 2 * pr
            p.op("pe", MMS([(po[:, qs * 256:qs * 256 + W], pt[:, kk * 512 + qs * 128:kk * 512 + (qs + 1) * 128],
                             Vt[:, kt0 + kk, 0:W], pr == 0 and kk == 0 and qs % 2 == 0, pr == npairs - 1 and kk == 1)
                            for kk in range(2) for qs in range(4)]),
                 reads=[Bpt, BV], writes=[Bpo])
            if pr != npairs - 1:
                return
            pov = po[:].rearrange("p (q w) -> p q w", w=256)
            if moba:
                p.op("dve", (lambda o, i_: (lambda e: e.reciprocal(out=o, in_=i_)))(rinv[:], pov[:, :, 64]),
                     reads=[Bpo], writes=[Brinv])
                om, Bom = omst[i % 2], Bomst[i % 2]
                p.op("dve", TT(om[:], pov[:, :, 0:64], bc(rinv[:].unsqueeze(2), [128, 4, 64]), ALU.mult),
                     reads=[Bpo, Brinv], writes=[Bom])
                p.dma("pool", DMA(OMv[:, i * 4:(i + 1) * 4, hi * 64:(hi + 1) * 64], om[:]), reads=[Bom], writes=[BOM], waw=False)
            else:
                p.op("dve", (lambda o, i_: (lambda e: e.reciprocal(out=o, in_=i_)))(rinv[:], pov[:, :, 128]),
                     reads=[Bpo], writes=[Brinv])
                if cc == 0:
                    p.op("dve", TT(o1s[:, i * 4:(i + 1) * 4, :], pov[:, :, 0:128], bc(rinv[:].unsqueeze(2), [128, 4, 128]),
                                   ALU.mult), reads=[Bpo, Brinv], writes=[Bo1s])
                else:
                    p.op("dve", TT(o2t[:], pov[:, :, 0:128], bc(rinv[:].unsqueeze(2), [128, 4, 128]), ALU.mult),
                         reads=[Bpo, Brinv], writes=[Bo2t])
                    p.op("dve", STT(odt[:], o2t[:], nlam[:, 0:1], o1s[:, i * 4:(i + 1) * 4, :], ALU.mult, ALU.add),
                         reads=[Bo2t, Bo1s], writes=[Bodt])
                    p.op("pool", TT(sq4[:], odt[:], odt[:], ALU.mult), reads=[Bodt], writes=[Bsq4])
                    p.op("dve", RED(st4[:, 0:4], sq4[:], ALU.add), reads=[Bsq4], writes=[Bst4])
                    p.op("dve", TS(st4[:, 0:4], st4[:, 0:4], 1.0 / 128, EPS, ALU.mult, ALU.add), reads=[Bst4], writes=[Bst4])
                    p.op("pool", TT(st4[:, 4:8], st4[:, 0:4], nhalf[:, 0:4], ALU.pow), reads=[Bst4], writes=[Bst4])
                    p.op("dve", TT(odt[:], odt[:], bc(st4[:, 4:8].unsqueeze(2), [128, 4, 128]), ALU.mult),
                         reads=[Bst4], writes=[Bodt])
                    od_, Bod_ = odst[i % 2], Bodst[i % 2]
                    p.op("dve", TT(od_[:], odt[:], bc(subg_sb[:].unsqueeze(1), [128, 4, 128]), ALU.mult),
                         reads=[Bodt], writes=[Bod_])
                    p.dma("pool", DMA(ODv[:, i * 4:(i + 1) * 4, hd * 128:(hd + 1) * 128], od_[:]), reads=[Bod_],
                          writes=[BOD], waw=False)

        load_head(head_list[0])
        emit_qk(0)
        for n in range(N):
            if n + 1 < N:
                emit_qk(n + 1)
            emit_exp(n)
            emit_av(n)
        p.barrier()
        p.emit()

    if stage < 4:
        return finish(nc, p, top, out_h, BOUT)

    with ExitStack() as es:
        wg8 = sbt(es, "wg8", [128, 8, 2048], BF16)
        wpm = sbt(es, "wpm", [128, 4, D], BF16)
        wpd = sbt(es, "wpd", [128, 4, D], BF16)
        wo = sbt(es, "wo", [128, 8, D], BF16)
        hTg = [sbt(es, "hTg%d" % i, [128, 8, 512], BF16) for i in range(2)]
        omt = [sbt(es, "omt%d" % i, [128, 4, 512], BF16) for i in range(2)]
        odt_ = [sbt(es, "odt%d" % i, [128, 4, 512], BF16) for i in range(2)]
        xg = [sbt(es, "xg%d" % i, [128, D], F32) for i in range(2)]
        omT = sbt(es, "omT", [128, 4, 512], BF16)
        odT = sbt(es, "odT", [128, 4, 512], BF16)
        sg = [sbt(es, "sg%d" % i, [128, 1024], F32) for i in range(2)]
        mT = sbt(es, "mT", [128, 8, 512], BF16)
        x1t = [sbt(es, "x1t%d" % i, [128, D], F32) for i in range(2)]
        sqj = sbt(es, "sqj", [128, D], BF16)
        ss = [sbt(es, "ss%d" % i, [128, 4], F32) for i in range(2)]
        h2 = [sbt(es, "h2%d" % i, [128, D], F32) for i in range(2)]
        h2b = [sbt(es, "h2b%d" % i, [128, D], BF16) for i in range(2)]
        h2T = sbt(es, "h2T", [128, 8, 128], F32)
        lg = sbt(es, "lg", [128, 36], F32)
        rs = sbt(es, "rs", [128, 32], F32)
        gsm = sbt(es, "gsm", [128, 16], F32)
        elm = sbt(es, "elm", [128, 32], F32)
        t8 = sbt(es, "t8", [128, 8], F32)
        oh1 = sbt(es, "oh1", [128, 32], F32)
        ohM = sbt(es, "ohM", [128, 32], F32)
        oh2 = sbt(es, "oh2", [128, 32], F32)
        ohb = sbt(es, "ohb", [128, 32], BF16)
        rank = sbt(es, "rank", [128, 32], F32)
        tq = sbt(es, "tq", [128, 32], F32)
        pT = pst(es, "pT", [128, 2048], BF16)
        pY = pst(es, "pY", [128, 1024], F32)
        pG = pst(es, "pG", [128, 1024], F32)
        pZ = pst(es, "pZ", [128, 1024], F32)
        (Bwg8, Bwpm, Bwpd, Bwo, BomT, BodT, Btm, BmT, Bsqj, Bh2T, Blg, Brs, Bgsm, Belm, Bt8, Boh1, BohM, Boh2, Bohb,
         Brank, Btq, BpT, BpY, BpG, BpZ, Bcarry, Bpos, Bwts) = [Buf(n) for n in (
            "wg8", "wpm", "wpd", "wo", "omT", "odT", "tm", "mT", "sqj", "h2T", "lg", "rs", "gsm", "elm", "t8", "oh1",
            "ohM", "oh2", "ohb", "rank", "tq", "pT", "pY", "pG", "pZ", "carry", "pos", "wts")]
        BhTg = [Buf("hTg%d" % i) for i in range(2)]
        Bomt = [Buf("omt%d" % i) for i in range(2)]
        Bodt_ = [Buf("odt%d" % i) for i in range(2)]
        Bxg = [Buf("xg%d" % i) for i in range(2)]
        Bsg = [Buf("sg%d" % i) for i in range(2)]
        Bx1t = [Buf("x1t%d" % i) for i in range(2)]
        Bss = [Buf("ss%d" % i) for i in range(2)]
        Bh2 = [Buf("h2%d" % i) for i in range(2)]
        Bh2b = [Buf("h2b%d" % i) for i in range(2)]

        for gi in range(4):
            p.dma("pool", DMA(wg8[:, :, gi * 512:(gi + 1) * 512],
                              w_in[:, 3072 + gi * 512:3072 + (gi + 1) * 512].rearrange("(k p) n -> p k n", p=128)),
                  writes=[Bwg8], waw=False)
        p.dma("pool", DMA(wpm[:], w_pm.rearrange("(k p) n -> p k n", p=128)), writes=[Bwpm])
        p.dma("pool", DMA(wpd[:], w_pd.rearrange("(k p) n -> p k n", p=128)), writes=[Bwpd])
        for gi in range(2):
            p.dma("pool", DMA(wo[:, :, gi * 512:(gi + 1) * 512], w_o[:, gi * 512:(gi + 1) * 512].rearrange("(k p) n -> p k n", p=128)),
                  writes=[Bwo], waw=False)
        for k in range(8):
            p.op("pool", TT(wo[:, k, :], wo[:, k, :], g1_bc[:], ALU.mult), reads=[Bwo], writes=[Bwo])
        OMr = OM.rearrange("(n p) c -> p n c", p=128)
        ODr = OD.rearrange("(n p) c -> p n c", p=128)
        XOr = xown.rearrange("(n p) c -> p n c", p=128)
        X1r = X1.rearrange("(n p) c -> p n c", p=128)

        def load_group(g):
            u = g % 2
            p.dma("sp", DMA(hTg[u][:], HT[:, :, g * 512:(g + 1) * 512]), reads=[BHT], writes=[BhTg[u]])
            p.dma("sp", DMA(omt[u][:], OMr[:, g * 4:(g + 1) * 4, :]), reads=[BOM], writes=[Bomt[u]])
            p.dma("sp", DMA(odt_[u][:], ODr[:, g * 4:(g + 1) * 4, :]), reads=[BOD], writes=[Bodt_[u]])

        h2b3 = h2b + [sbt(es, "h2b2", [128, D], BF16)]
        Bh2b3 = Bh2b + [Buf("h2b2")]
        lg2 = [lg, sbt(es, "lg1", [128, 36], F32)]
        Blg2 = [Blg, Buf("lg1")]
        pTv = pT[:].rearrange("p (c n) -> p c n", n=512)

        def g_pre(g):
            if g + 1 < 8:
                load_group(g + 1)
            u = g % 2
            p.op("pe", TRS([(pTv[:, cb, qs * 128:(qs + 1) * 128], omt[u][:, qs, cb * 128:(cb + 1) * 128], ident_b[:])
                            for qs in range(4) for cb in range(4)]), reads=[Bomt[u]], writes=[BpT])
            p.op("act", ACPY(omT[:], pTv), reads=[BpT], writes=[BomT])
            p.op("pe", TRS([(pTv[:, cb, qs * 128:(qs + 1) * 128], odt_[u][:, qs, cb * 128:(cb + 1) * 128], ident_b[:])
                            for qs in range(4) for cb in range(4)]), reads=[Bodt_[u]], writes=[BpT])
            p.op("dve", CPY(odT[:], pTv), reads=[BpT], writes=[BodT])
            for oc in range(8):
                osl = slice(oc * 128, (oc + 1) * 128)
                p.op("pe", MMS([(pG[:, 0:512], wg8[:, k, oc * 128:(oc + 1) * 128], hTg[u][:, k, :], k == 0, k == 7)
                                for k in range(8)] +
                               [(pG[:, 512:1024], wg8[:, k, 1024 + oc * 128:1024 + (oc + 1) * 128], hTg[u][:, k, :], k == 0, k == 7)
                                for k in range(8)]), reads=[Bwg8, BhTg[u]], writes=[BpG])
                p.op("pe", MMS([(pY[:, 0:512], wpm[:, kc, osl], omT[:, kc, :], kc == 0, kc == 3) for kc in range(4)] +
                               [(pY[:, 512:1024], wpd[:, kc, osl], odT[:, kc, :], kc == 0, kc == 3) for kc in range(4)]),
                     reads=[Bwpm, Bwpd, BomT, BodT], writes=[BpY])
                sgu, Bsgu = sg[oc % 2], Bsg[oc % 2]
                p.op("act", ACTF(sgu[:], pG[:], AF.Sigmoid), reads=[BpG], writes=[Bsgu])
                p.op("dve", TT(sgu[:], pY[:], sgu[:], ALU.mult), reads=[BpY], writes=[Bsgu])
                p.op("pool", TT(mT[:, oc, :], sgu[:, 0:512], sgu[:, 512:1024], ALU.add), reads=[Bsgu], writes=[BmT])

        def s0(t):
            g, qs = t // 4, t % 4
            if qs == 0:
                g_pre(g)
            v = t % 2
            p.dma("sp", DMA(xg[v][:], XOr[:, t, :]), writes=[Bxg[v]])
            p.op("pe", MMS([(pZ[:, nh * 512:(nh + 1) * 512], mT[:, k, qs * 128:(qs + 1) * 128], wo[:, k, nh * 512:(nh + 1) * 512],
                             k == 0, k == 7) for k in range(8) for nh in range(2)]), reads=[BmT, Bwo], writes=[BpZ])
            p.op("dve", TT(x1t[v][:], pZ[:], xg[v][:], ALU.add), reads=[BpZ, Bxg[v]], writes=[Bx1t[v]])
            p.dma("act", DMA(X1r[:, t, :], x1t[v][:]), reads=[Bx1t[v]], writes=[BX1], waw=False)
            p.op("act", ACTF(sqj[:], x1t[v][:], AF.Square, accum=ss[v][:, 0:1]), reads=[Bx1t[v]], writes=[Bsqj, Bss[v]])
            p.op("dve", TS(ss[v][:, 1:2], ss[v][:, 0:1], 1.0 / D, EPS, ALU.mult, ALU.add), reads=[Bss[v]], writes=[Bss[v]])
            p.op("pool", TT(ss[v][:, 2:3], ss[v][:, 1:2], nhalf[:, 0:1], ALU.pow), reads=[Bss[v]], writes=[Bss[v]])
            p.op("dve", STT(h2[v][:], x1t[v][:], ss[v][:, 2:3], a2_bc[:], ALU.mult, ALU.mult), reads=[Bx1t[v], Bss[v]],
                 writes=[Bh2[v]])
            p.op("dve", TT(h2[v][:], h2[v][:], b2_bc[:], ALU.add), reads=[Bh2[v]], writes=[Bh2[v]])
            p.op("act", ACPY(h2b3[t % 3][:], h2[v][:]), reads=[Bh2[v]], writes=[Bh2b3[t % 3]])

        def s1(t):
            v = t % 2
            lgv, Blgv = lg2[v], Blg2[v]
            p.op("pe", TRS([(pG[:, k * 128:(k + 1) * 128], h2[v][:, k * 128:(k + 1) * 128], ident_f[:]) for k in range(8)]),
                 reads=[Bh2[v]], writes=[BpG])
            p.op("act", ACPY(h2T[:], pG[:].rearrange("p (k n) -> p k n", n=128)), reads=[BpG], writes=[Bh2T])
            p.op("pe", MMS([(pY[:, 0:36], h2T[:, k, :], wr_sb[:, k, :], k == 0, k == 7) for k in range(8)]),
                 reads=[Bh2T], writes=[BpY])
            p.op("dve", TT(lgv[:], pY[:, 0:36], br_sb[:], ALU.add), reads=[BpY], writes=[Blgv])

        def s2(t):
            v = t % 2
            lg, Blg = lg2[v], Blg2[v]
            p.op("dve", RED(rs[:, 0:1], lg[:, 0:4], ALU.max), reads=[Blg], writes=[Brs])
            p.op("dve", TS(gsm[:, 0:4], lg[:, 0:4], rs[:, 0:1], None, ALU.subtract), reads=[Blg, Brs], writes=[Bgsm])
            p.op("act", ACTF(gsm[:, 4:8], gsm[:, 0:4], AF.Sigmoid), reads=[Bgsm], writes=[Bgsm])
            p.op("act", ACTF(gsm[:, 8:12], gsm[:, 0:4], AF.Sigmoid, scale=-1.0), reads=[Bgsm], writes=[Bgsm])
            p.op("dve", (lambda o, i_: (lambda e: e.reciprocal(out=o, in_=i_)))(gsm[:, 12:16], gsm[:, 8:12]),
                 reads=[Bgsm], writes=[Bgsm])
            p.op("dve", TT(gsm[:, 4:8], gsm[:, 4:8], gsm[:, 12:16], ALU.mult), reads=[Bgsm], writes=[Bgsm])
            p.op("dve", RED(rs[:, 1:2], gsm[:, 4:8], ALU.add), reads=[Bgsm], writes=[Brs])
            p.op("dve", (lambda o, i_: (lambda e: e.reciprocal(out=o, in_=i_)))(rs[:, 2:3], rs[:, 1:2]),
                 reads=[Brs], writes=[Brs])
            p.op("dve", TS(gsm[:, 0:4], lg[:, 0:4], rs[:, 0:1], None, ALU.is_ge), reads=[Blg, Brs], writes=[Bgsm])
            p.op("dve", TS(gsm[:, 0:4], gsm[:, 0:4], 1e30, -1e30, ALU.mult, ALU.add), reads=[Bgsm], writes=[Bgsm])
            p.op("dve", TT(elm[:].rearrange("p (g e) -> p g e", e=8), lg[:, 4:36].rearrange("p (g e) -> p g e", e=8),
                           bc(gsm[:, 0:4].unsqueeze(2), [128, 4, 8]), ALU.add), reads=[Blg, Bgsm], writes=[Belm])
            p.op("dve", (lambda o, i_: (lambda e: e.max(out=o, in_=i_)))(t8[:], elm[:]), reads=[Belm], writes=[Bt8])
            p.op("dve", TS(oh1[:], elm[:], t8[:, 0:1], None, ALU.is_equal), reads=[Belm, Bt8], writes=[Boh1])
            p.op("dve", TS(ohM[:], elm[:], t8[:, 1:2], None, ALU.is_ge), reads=[Belm, Bt8], writes=[BohM])
            p.op("dve", TT(oh2[:], ohM[:], oh1[:], ALU.subtract), reads=[BohM, Boh1], writes=[Boh2])
            p.op("dve", TT(rs[:, 3:4], t8[:, 0:1], t8[:, 1:2], ALU.subtract), reads=[Bt8], writes=[Brs])
            p.op("act", ACTF(rs[:, 4:5], rs[:, 3:4], AF.Sigmoid), reads=[Brs], writes=[Brs])
            p.op("dve", TS(rs[:, 5:6], rs[:, 4:5], -1.0, 1.0, ALU.mult, ALU.add), reads=[Brs], writes=[Brs])
            p.op("dve", TS(wts_sb[:, t, :], rs[:, 4:6], rs[:, 2:3], None, ALU.mult), reads=[Brs], writes=[Bwts])
            p.op("dve", CPY(ohb[:], ohM[:]), reads=[BohM], writes=[Bohb])
            p.op("pe", MMS([(pY[:, 64:96], utri_b[:], ohb[:], True, True), (pY[:, 96:128], ones_b[:], ohb[:], True, True)]),
                 reads=[Bohb], writes=[BpY])
            p.op("dve", TT(rank[:], pY[:, 64:96], carry[:], ALU.add), reads=[BpY, Bcarry], writes=[Brank])
            p.op("dve", TT(carry[:], carry[:], pY[:, 96:128], ALU.add), reads=[BpY, Brank], writes=[Bcarry])
            for j, oh in enumerate((oh1, oh2)):
                Boh = (Boh1, Boh2)[j]
                b0 = 8 + j * 8
                p.op("dve", TT(tq[:], oh[:], rank[:], ALU.mult), reads=[Boh, Brank], writes=[Btq])
                p.op("dve", RED(rs[:, b0:b0 + 1], tq[:], ALU.add), reads=[Btq], writes=[Brs])
                p.op("dve", TT(tq[:], oh[:], ebase_sb[:], ALU.mult), reads=[Boh, Brs], writes=[Btq])
                p.op("dve", RED(rs[:, b0 + 1:b0 + 2], tq[:], ALU.add), reads=[Btq], writes=[Brs])
                p.op("dve", TS(rs[:, b0 + 2:b0 + 3], rs[:, b0:b0 + 1], float(cap) - 0.5, 1e6, ALU.is_ge, ALU.mult),
                     reads=[Brs], writes=[Brs])
                p.op("dve", TT(rs[:, b0 + 3:b0 + 4], rs[:, b0:b0 + 1], rs[:, b0 + 1:b0 + 2], ALU.add), reads=[Brs], writes=[Brs])
                p.op("dve", TT(rs[:, b0 + 3:b0 + 4], rs[:, b0 + 3:b0 + 4], rs[:, b0 + 2:b0 + 3], ALU.add), reads=[Brs], writes=[Brs])
                p.op("dve", CPY(pos_i[:, t, j:j + 1], rs[:, b0 + 3:b0 + 4]), reads=[Brs], writes=[Bpos])
                p.dma("pool", (lambda idx, src: (lambda e: e.indirect_dma_start(
                    out=XS, out_offset=bass.IndirectOffsetOnAxis(ap=idx, axis=0), in_=src, in_offset=None,
                    bounds_check=bnd(e, "p3"), oob_is_err=False)))(pos_i[:, t, j:j + 1], h2b3[t % 3][:, :]),
                    reads=[Bpos, Bh2b3[t % 3]], writes=[BXS], waw=False)

        load_group(0)
        pipeline(NT_OWN, [s0, s1, s2])
        p.barrier()
        p.emit()

    if stage < 5:
        return finish(nc, p, top, out_h, BOUT)

    with ExitStack() as es:
        wgs = [sbt(es, "wgs%d" % i, [128, 8, 512], BF16) for i in range(2)]
        wus = [sbt(es, "wus%d" % i, [128, 8, 512], BF16) for i in range(2)]
        wds = [sbt(es, "wds%d" % i, [128, 4, D], BF16) for i in range(2)]
        xrow = [sbt(es, "xrow%d" % i, [128, 4, D], BF16) for i in range(2)]
        xsT = [sbt(es, "xsT%d" % i, [128, 8, 512], BF16) for i in range(2)]
        sgm = [sbt(es, "sgm%d" % i, [128, 512], F32) for i in range(2)]
        hidT = [sbt(es, "hidT%d" % i, [128, 4, 512], BF16) for i in range(2)]
        yrow = [sbt(es, "yrow%d" % i, [128, D], F32) for i in range(2)]
        pT = pst(es, "pT", [128, 2048], BF16)
        pGU = [pst(es, "pGU%d" % i, [128, 1024], F32) for i in range(2)]
        pOu = pst(es, "pOu", [128, 1024], F32)
        Bwgs = [Buf("wgs%d" % i) for i in range(2)]
        Bwus = [Buf("wus%d" % i) for i in range(2)]
        Bwds = [Buf("wds%d" % i) for i in range(2)]
        Bxrow = [Buf("xrow%d" % i) for i in range(2)]
        BxsT = [Buf("xsT%d" % i) for i in range(2)]
        Bsgm = [Buf("sgm%d" % i) for i in range(2)]
        BhidT = [Buf("hidT%d" % i) for i in range(2)]
        Byrow = [Buf("yrow%d" % i) for i in range(2)]
        BpT = Buf("pT")
        BpGU = [Buf("pGU%d" % i) for i in range(2)]
        BpOu = Buf("pOu")

        groups = []
        r0 = 0
        while r0 < cap:
            R = min(512, cap - r0)
            groups.append((r0, R))
            r0 += R

        def load_w(e):
            u = e % 2
            p.dma("pool", DMA(wgs[u][:], w_gate[e].rearrange("(k p) n -> p k n", p=128)), writes=[Bwgs[u]])
            p.dma("pool", DMA(wus[u][:], w_up[e].rearrange("(k p) n -> p k n", p=128)), writes=[Bwus[u]])
            p.dma("pool", DMA(wds[u][:], w_down[e].rearrange("(k p) n -> p k n", p=128)), writes=[Bwds[u]])

        xrow3 = xrow + [sbt(es, "xrow2", [128, 4, D], BF16)]
        Bxrow3 = Bxrow + [Buf("xrow2")]
        gitems = [(e, r0, R, gidx == 0) for e in range(32) for gidx, (r0, R) in enumerate(groups)]
        assert len(groups) >= 2
        pTv = pT[:].rearrange("p (k n) -> p k n", n=512)

        def e0(gi):
            e, r0, R, first = gitems[gi]
            nt = R // 128
            base = e * cap + r0
            p.dma("sp", DMA(xrow3[gi % 3][:, 0:nt, :], XS[base:base + R, :].rearrange("(n p) d -> p n d", p=128)),
                  reads=[BXS], writes=[Bxrow3[gi % 3]])

        def e1(gi):
            e, r0, R, first = gitems[gi]
            nt = R // 128
            v = gi % 2
            xr, Bxr = xrow3[gi % 3], Bxrow3[gi % 3]
            for half in range(2):
                p.op("pe", TRS([(pTv[:, kk, n * 128:(n + 1) * 128], xr[:, n, (half * 4 + kk) * 128:(half * 4 + kk + 1) * 128],
                                 ident_b[:]) for n in range(nt) for kk in range(4)]), reads=[Bxr], writes=[BpT])
                if half == 0:
                    p.op("act", ACPY(xsT[v][:, 0:4, 0:R], pTv[:, :, 0:R]), reads=[BpT], writes=[BxsT[v]])
                else:
                    p.op("dve", CPY(xsT[v][:, 4:8, 0:R], pTv[:, :, 0:R]), reads=[BpT], writes=[BxsT[v]])

        def e2(gi):
            e, r0, R, first = gitems[gi]
            u = e % 2
            v = gi % 2
            for hc in range(4):
                w_ = hc % 2
                hsl = slice(hc * 128, (hc + 1) * 128)
                p.op("pe", MMS([(pGU[w_][:, 0:R], wgs[u][:, k, hsl], xsT[v][:, k, 0:R], k == 0, k == 7) for k in range(8)] +
                               [(pGU[w_][:, 512:512 + R], wus[u][:, k, hsl], xsT[v][:, k, 0:R], k == 0, k == 7) for k in range(8)]),
                     reads=[Bwgs[u], Bwus[u], BxsT[v]], writes=[BpGU[w_]])
                p.op("act", ACTF(sgm[w_][:, 0:R], pGU[w_][:, 0:R], AF.Silu), reads=[BpGU[w_]], writes=[Bsgm[w_]])
                p.op("dve", TT(hidT[v][:, hc, 0:R], sgm[w_][:, 0:R], pGU[w_][:, 512:512 + R], ALU.mult),
                     reads=[Bsgm[w_], BpGU[w_]], writes=[BhidT[v]])

        ycount = [0]

        def e3(gi):
            e, r0, R, first = gitems[gi]
            u = e % 2
            v = gi % 2
            nt = R // 128
            base = e * cap + r0
            if first and e + 1 < 32:
                load_w(e + 1)
            for n in range(nt):
                y_ = ycount[0] % 2
                ycount[0] += 1
                p.op("pe", MMS([(pOu[:, nh * 512:(nh + 1) * 512], hidT[v][:, hc, n * 128:(n + 1) * 128],
                                 wds[u][:, hc, nh * 512:(nh + 1) * 512], hc == 0, hc == 3) for hc in range(4) for nh in range(2)]),
                     reads=[BhidT[v], Bwds[u]], writes=[BpOu])
                p.op("act", ACPY(yrow[y_][:, 0:512], pOu[:, 0:512]), reads=[BpOu], writes=[Byrow[y_]])
                p.op("dve", CPY(yrow[y_][:, 512:1024], pOu[:, 512:1024]), reads=[BpOu], writes=[Byrow[y_]])
                p.dma("sp", DMA(YS[base + n * 128:base + (n + 1) * 128, :], yrow[y_][:]), reads=[Byrow[y_]], writes=[BYS], waw=False)

        load_w(0)
        pipeline(len(gitems), [e0, e1, e2, e3])
        p.barrier()
        p.emit()

    with ExitStack() as es:
        x1l = [sbt(es, "x1l%d" % i, [128, D], F32) for i in range(2)]
        r1 = [sbt(es, "r1%d" % i, [128, D], F32) for i in range(2)]
        r2 = [sbt(es, "r2%d" % i, [128, D], F32) for i in range(2)]
        yt = [sbt(es, "yt%d" % i, [128, D], F32) for i in range(2)]
        ot = [sbt(es, "ot%d" % i, [128, D], F32) for i in range(2)]
        sqj = sbt(es, "sqj", [128, D], BF16)
        ss = [sbt(es, "ss%d" % i, [128, 4], F32) for i in range(2)]
        Bx1l = [Buf("x1l%d" % i) for i in range(2)]
        Br1 = [Buf("r1%d" % i) for i in range(2)]
        Br2 = [Buf("r2%d" % i) for i in range(2)]
        Byt = [Buf("yt%d" % i) for i in range(2)]
        Bot = [Buf("ot%d" % i) for i in range(2)]
        Bss = [Buf("ss%d" % i) for i in range(2)]
        Bsqj = Buf("sqj")
        X1r = X1.rearrange("(n p) c -> p n c", p=128)
        OUTr = out_h.rearrange("(n p) c -> p n c", p=128)
        x1l3 = x1l + [sbt(es, "x1l2", [128, D], F32)]
        r13 = r1 + [sbt(es, "r12", [128, D], F32)]
        r23 = r2 + [sbt(es, "r22", [128, D], F32)]
        Bx1l3 = Bx1l + [Buf("x1l2")]
        Br13 = Br1 + [Buf("r12")]
        Br23 = Br2 + [Buf("r22")]

        def f0(t):
            w3 = t % 3
            p.dma("sp", DMA(x1l3[w3][:], X1r[:, t, :]), reads=[BX1], writes=[Bx1l3[w3]])
            for j, (rr, Brr) in enumerate(((r13[w3], Br13[w3]), (r23[w3], Br23[w3]))):
                p.op("pool", MSET(rr[:], 0.0), writes=[Brr])
                p.dma("pool", (lambda idx, dst: (lambda e: e.indirect_dma_start(
                    out=dst, out_offset=None, in_=YS, in_offset=bass.IndirectOffsetOnAxis(ap=idx, axis=0),
                    bounds_check=bnd(e, "p5"), oob_is_err=False)))(pos_i[:, t, j:j + 1], rr[:, :]),
                    reads=[BYS], writes=[Brr])

        def f1(t):
            v = t % 2
            w3 = t % 3
            p.op("dve", TS(yt[v][:], r13[w3][:], wts_sb[:, t, 0:1], None, ALU.mult), reads=[Br13[w3]], writes=[Byt[v]])
            p.op("dve", STT(yt[v][:], r23[w3][:], wts_sb[:, t, 1:2], yt[v][:], ALU.mult, ALU.add), reads=[Br23[w3]], writes=[Byt[v]])
            p.op("dve", TT(yt[v][:], yt[v][:], g2_bc[:], ALU.mult), reads=[Byt[v]], writes=[Byt[v]])
            p.op("dve", TT(yt[v][:], yt[v][:], x1l3[w3][:], ALU.add), reads=[Bx1l3[w3]], writes=[Byt[v]])
            p.op("act", ACTF(sqj[:], yt[v][:], AF.Square, accum=ss[v][:, 0:1]), reads=[Byt[v]], writes=[Bsqj, Bss[v]])
            p.op("dve", TS(ss[v][:, 1:2], ss[v][:, 0:1], 1.0 / D, EPS, ALU.mult, ALU.add), reads=[Bss[v]], writes=[Bss[v]])
            p.op("pool", TT(ss[v][:, 2:3], ss[v][:, 1:2], nhalf[:, 0:1], ALU.pow), reads=[Bss[v]], writes=[Bss[v]])
            p.op("dve", STT(ot[v][:], yt[v][:], ss[v][:, 2:3], fing_sb[:], ALU.mult, ALU.mult), reads=[Byt[v], Bss[v]], writes=[Bot[v]])
            p.dma("sp", DMA(OUTr[:, t, :], ot[v][:]), reads=[Bot[v]], writes=[BOUT], waw=False)

        pipeline(NT_OWN, [f0, f1])
        p.barrier()
        p.emit()

    return finish(nc, p, top, out_h, BOUT)


def finish(nc, p, top, out_h, BOUT):
    p.barrier()
    p.emit()
    p.close()
    top.close()
    return nc


def host_prep(inputs, cap=CAP):
    x = np.ascontiguousarray(inputs["x"], dtype=np.float32)
    c = inputs["c"]
    f32 = np.float32

    def rep(v, n=128):
        return np.ascontiguousarray(np.broadcast_to(np.asarray(v, dtype=f32).reshape(1, -1), (n, np.asarray(v).size)))

    inv = 1.0 / (10000.0 ** (np.arange(0, 64, 2, dtype=np.float32) / 64.0))
    ang = np.arange(S, dtype=np.float32)[:, None] * inv[None, :].astype(np.float32)
    cs = np.concatenate([np.cos(ang), np.sin(ang)], axis=1).astype(f32)
    khot = np.zeros((33, S), f32)
    for n in range(32):
        khot[n, n * 256:(n + 1) * 256] = 1.0
    khot[32, :] = 1.0
    kk = np.arange(128)[:, None]
    qq = np.arange(512)[None, :]
    diag = [((j * 128 + kk) <= qq).astype(f32) for j in range(4)]
    ones = np.ones((128, 512), f32)
    zeros = np.zeros((128, 512), f32)
    patA = diag + [zeros] * 4
    patB = [ones] * 4 + diag
    ident = np.eye(128, dtype=f32)
    utri = (np.arange(128)[:, None] < np.arange(128)[None, :]).astype(f32)
    lam4 = np.stack([rep(inputs[k][0]) for k in ("lambda_q1", "lambda_k1", "lambda_q2", "lambda_k2")], axis=1)
    shared = dict(
        w_ada=np.ascontiguousarray(inputs["w_ada"][0]), bada_bc=rep(inputs["b_ada"][0]),
        n1g_bc=rep(inputs["norm1_g"][0]), n2g_bc=rep(inputs["norm2_g"][0]), fing_bc=rep(inputs["final_g"]),
        w_in=np.ascontiguousarray(inputs["w_in"][0]), khot=khot, ident=ident, utri=utri,
        lam4=np.ascontiguousarray(lam4), subg_bc=rep(inputs["diff_subln_g"][0]),
        w_pm=np.ascontiguousarray(inputs["w_proj_moba"][0]), w_pd=np.ascontiguousarray(inputs["w_proj_diff"][0]),
        w_o=np.ascontiguousarray(inputs["w_out"][0]),
        w_r=np.ascontiguousarray(np.concatenate([inputs["w_group"][0], inputs["w_expert"][0]], axis=1)),
        br_bc=rep(np.concatenate([inputs["b_group"][0], inputs["b_expert"][0]])),
        ebase_bc=rep(np.arange(32, dtype=f32) * cap),
        w_gate=np.ascontiguousarray(inputs["w_gate"][0]), w_up=np.ascontiguousarray(inputs["w_up"][0]),
        w_down=np.ascontiguousarray(inputs["w_down"][0]),
        cs_all=np.ascontiguousarray(cs.reshape(NT_ALL, 128, 64).transpose(1, 0, 2)),
    )
    in_maps, own_idx = [], []
    for core in range(8):
        b, pp = core // 2, core % 2
        chunks = OWN_CHUNKS[pp]
        idx = np.concatenate([np.arange(ch * 512, (ch + 1) * 512) for ch in chunks])
        own_idx.append((b, idx))
        pats = []
        for par in range(2):
            cch = chunks[par]
            pats.append(patA if cch % 2 == 0 else patB)
        cmask = np.stack(pats[0] + pats[1], axis=0)
        blk = idx // 256
        nn = np.arange(32)[None, :]
        bval = np.where(nn < blk[:, None], 0.0, -1e30).astype(f32)
        oblk = (nn == blk[:, None]).astype(f32)
        m = dict(shared)
        m.update(
            xall=x[b], xown=np.ascontiguousarray(x[b][idx]),
            c_col=np.ascontiguousarray(c[b].reshape(8, 128).T.astype(f32)),
            cs_own=np.ascontiguousarray(cs[idx].reshape(NT_OWN, 128, 64).transpose(1, 0, 2)),
            cmask=np.ascontiguousarray(cmask.transpose(1, 0, 2)),
            blkvalid=np.ascontiguousarray(bval.reshape(NT_OWN, 128, 32).transpose(1, 0, 2)),
            ownblk=np.ascontiguousarray(oblk.reshape(NT_OWN, 128, 32).transpose(1, 0, 2)),
        )
        in_maps.append(m)
    return in_maps, own_idx


def kernel(**inputs):
    in_maps, own_idx = host_prep(inputs)
    nc = build()
    res = run_bass_kernel_spmd(nc, in_maps, core_ids=list(range(8)))
    out = np.empty((4, S, D), np.float32)
    for core in range(8):
        b, idx = own_idx[core]
        out[b, idx, :] = res.results[core]["out"]
    return out
```

```python
import numpy as np
from contextlib import ExitStack
import concourse.bass as bass
import concourse.mybir as mybir
from concourse.bass_utils import run_bass_kernel_spmd

F32 = mybir.dt.float32
BF16 = mybir.dt.bfloat16
I32 = mybir.dt.int32
ALU = mybir.AluOpType
AF = mybir.ActivationFunctionType
AX = mybir.AxisListType

S = 8192
D = 1024
NO = 4096
NT_ALL = 64
NT_OWN = 32
CAP = 1024
EPS = 1e-6
OWN_CHUNKS = ([0, 3, 4, 7, 8, 11, 12, 15], [1, 2, 5, 6, 9, 10, 13, 14])


class Buf:
    __slots__ = ("name", "w", "r", "dsem", "dcnt")

    def __init__(self, name):
        self.name = name
        self.w = None
        self.r = {}
        self.dsem = None
        self.dcnt = 0


class Prog:
    ENGS = ("pe", "act", "dve", "pool", "sp")

    def __init__(self, nc):
        self.nc = nc
        self.ops = {e: [] for e in self.ENGS}
        self.cnt = {e: 0 for e in self.ENGS}
        self.seen = {e: {} for e in self.ENGS}
        self.sems = {}
        self._stack = []
        self.dmabufs = []
        for e in ("pe", "act", "dve", "pool"):
            self.sems[e] = self.new_sem("prog_" + e)

    def new_sem(self, name):
        self.nsem = getattr(self, "nsem", 0) + 1
        cm = self.nc.semaphore("%s_%d" % (name, self.nsem))
        s = cm.__enter__()
        self._stack.append(cm)
        return s

    def close(self):
        for cm in reversed(self._stack):
            cm.__exit__(None, None, None)

    def _need(self, eng, ev, waits):
        if ev is None:
            return
        sem, val, _ = ev
        k = id(sem)
        if self.seen[eng].get(k, 0) >= val:
            return
        if k in waits and waits[k][1] >= val:
            return
        waits[k] = (sem, val)

    def _flush(self, eng, waits):
        for k, (sem, val) in waits.items():
            self.seen[eng][k] = val
            self.ops[eng].append(("wait", sem, val))

    def _deps(self, eng, reads, writes, waw=True):
        waits = {}
        skip_same = (eng == "pe")
        for b in reads:
            if b.w is not None and not (skip_same and b.w[2] == eng):
                self._need(eng, b.w, waits)
        for b in writes:
            if waw and b.w is not None and not (skip_same and b.w[2] == eng):
                self._need(eng, b.w, waits)
            for ev in b.r.values():
                if ev[2] == eng:
                    continue
                self._need(eng, ev, waits)
        self._flush(eng, waits)

    def _commit(self, ev, reads, writes):
        k = id(ev[0])
        for b in reads:
            old = b.r.get(k)
            if old is None or old[1] < ev[1]:
                b.r[k] = ev
        for b in writes:
            b.w = ev
            b.r = {}

    def op(self, eng, fn, reads=(), writes=()):
        self._deps(eng, reads, writes)
        self.cnt[eng] += 1
        ev = (self.sems[eng], self.cnt[eng], eng)
        self.ops[eng].append(("inst", fn, self.sems[eng], 1))
        self._commit(ev, reads, writes)

    def dma(self, eng, fn, reads=(), writes=(), waw=True):
        sb = writes[0]
        if sb.dsem is None:
            sb.dsem = self.new_sem("d_" + sb.name)
            self.dmabufs.append(sb)
        self._deps(eng, reads, writes, waw=waw)
        sb.dcnt += 16
        ev = (sb.dsem, sb.dcnt, "dma")
        self.ops[eng].append(("inst", fn, sb.dsem, 16))
        self._commit(ev, reads, writes)

    def barrier(self):
        for e in self.ENGS:
            waits = {}
            for o in ("pe", "act", "dve", "pool"):
                if o != e and self.cnt[o] > 0:
                    self._need(e, (self.sems[o], self.cnt[o], o), waits)
            for b in self.dmabufs:
                self._need(e, (b.dsem, b.dcnt, "dma"), waits)
            self._flush(e, waits)

    def emit(self):
        nc = self.nc
        eng_map = {"sp": "sync", "pe": "tensor", "act": "scalar", "dve": "vector", "pool": "gpsimd"}
        with nc.Block() as block:
            def mk(e):
                def run(engobj):
                    for item in self.ops[e]:
                        if item[0] == "wait":
                            engobj.wait_ge(item[1], item[2])
                        else:
                            inst = item[1](engobj)
                            if item[2] is not None:
                                inst.then_inc(item[2], item[3])
                return run
            for e in self.ENGS:
                getattr(block, eng_map[e])(mk(e))
        self.ops = {e: [] for e in self.ENGS}


def MM(out, lhsT, rhs, start=True, stop=True):
    return lambda e: e.matmul(out, lhsT=lhsT, rhs=rhs, start=start, stop=stop)


def MMS(lst):
    def f(e):
        r = None
        for (out, lhsT, rhs, st, sp) in lst:
            r = e.matmul(out, lhsT=lhsT, rhs=rhs, start=st, stop=sp)
        return r
    return f


def TRS(lst):
    def f(e):
        r = None
        for (out, in_, ident) in lst:
            r = e.transpose(out=out, in_=in_, identity=ident)
        return r
    return f


def ACTF(out, in_, func, bias=0.0, scale=1.0, accum=None):
    if accum is None:
        return lambda e: e.activation(out=out, in_=in_, func=func, bias=bias, scale=scale)
    return lambda e: e.activation(out=out, in_=in_, func=func, bias=bias, scale=scale, accum_out=accum)


def CPY(out, in_):
    return lambda e: e.tensor_copy(out=out, in_=in_)


def ACPY(out, in_):
    return lambda e: e.copy(out=out, in_=in_)


def TT(out, in0, in1, op):
    return lambda e: e.tensor_tensor(out=out, in0=in0, in1=in1, op=op)


def TS(out, in0, s1, s2, op0, op1=None):
    if op1 is None:
        return lambda e: e.tensor_scalar(out=out, in0=in0, scalar1=s1, scalar2=None, op0=op0)
    return lambda e: e.tensor_scalar(out=out, in0=in0, scalar1=s1, scalar2=s2, op0=op0, op1=op1)


def STT(out, in0, scalar, in1, op0, op1):
    return lambda e: e.scalar_tensor_tensor(out=out, in0=in0, scalar=scalar, in1=in1, op0=op0, op1=op1)


def RED(out, in_, op, axis=AX.X):
    return lambda e: e.tensor_reduce(out=out, in_=in_, axis=axis, op=op)


def MSET(ap, v):
    return lambda e: e.memset(ap, v)


def DMA(out, in_):
    return lambda e: e.dma_start(out=out, in_=in_)


def bc(ap, shape):
    return ap.to_broadcast(shape)


def build(stage=99, cap=CAP, dbg=False):
    nc = bass.Bass("TRN2", target_bir_lowering=False)

    def din(name, shape, dt=F32):
        return nc.dram_tensor(name, list(shape), dt, kind="ExternalInput").ap()

    def dscr(name, shape, dt):
        kind = "ExternalOutput" if dbg else "Internal"
        return nc.dram_tensor(name, list(shape), dt, kind=kind).ap()

    xall = din("xall", [S, D])
    xown = din("xown", [NO, D])
    c_col = din("c_col", [128, 8])
    w_ada = din("w_ada", [D, 6 * D])
    bada_bc = din("bada_bc", [128, 6 * D])
    n1g_bc = din("n1g_bc", [128, D])
    n2g_bc = din("n2g_bc", [128, D])
    fing_bc = din("fing_bc", [128, D])
    w_in = din("w_in", [D, 5120])
    cs_all = din("cs_all", [128, NT_ALL, 64])
    cs_own = din("cs_own", [128, NT_OWN, 64])
    khot = din("khot", [33, S])
    cmask = din("cmask", [128, 16, 512])
    blkvalid = din("blkvalid", [128, NT_OWN, 32])
    ownblk = din("ownblk", [128, NT_OWN, 32])
    ident_h = din("ident", [128, 128])
    utri_h = din("utri", [128, 128])
    lam4 = din("lam4", [128, 4, 64])
    subg_bc = din("subg_bc", [128, 128])
    w_pm = din("w_pm", [512, D])
    w_pd = din("w_pd", [512, D])
    w_o = din("w_o", [D, D])
    w_r = din("w_r", [D, 36])
    br_bc = din("br_bc", [128, 36])
    ebase_bc = din("ebase_bc", [128, 32])
    w_gate = din("w_gate", [32, D, 512])
    w_up = din("w_up", [32, D, 512])
    w_down = din("w_down", [32, 512, D])
    out_h = nc.dram_tensor("out", [NO, D], F32, kind="ExternalOutput").ap()

    KT2 = dscr("KT2", [8, 128, S], BF16)
    VS = dscr("VS", [S, D], BF16)
    QT = dscr("QT", [128, 16, NO], BF16)
    HT = dscr("HT", [128, 8, NO], BF16)
    OM = dscr("OM", [NO, 512], BF16)
    OD = dscr("OD", [NO, 512], BF16)
    X1 = dscr("X1", [NO, D], F32)
    XS = dscr("XS", [32 * cap, D], BF16)
    YS = dscr("YS", [32 * cap, D], F32)
    BKT, BVS, BQT, BHT, BOM, BOD, BX1, BXS, BYS, BOUT = [Buf(n) for n in
        ("KT2", "VS", "QT", "HT", "OM", "OD", "X1", "XS", "YS", "OUT")]

    p = Prog(nc)
    top = ExitStack()

    uid = [0]
    regcache = {}

    def bnd(e, key):
        if key not in regcache:
            regcache[key] = e.to_reg(32 * cap - 1)
        return regcache[key]

    def sbt(es, name, shape, dt):
        uid[0] += 1
        return es.enter_context(nc.sbuf_tensor("%s_%d" % (name, uid[0]), list(shape), dt))

    def pst(es, name, shape, dt):
        uid[0] += 1
        return es.enter_context(nc.psum_tensor("%s_%d" % (name, uid[0]), list(shape), dt))

    ident_f = sbt(top, "ident_f", [128, 128], F32)
    ident_b = sbt(top, "ident_b", [128, 128], BF16)
    ones_f = sbt(top, "ones_f", [128, 128], F32)
    ones_b = sbt(top, "ones_b", [128, 128], BF16)
    utri_b = sbt(top, "utri_b", [128, 128], BF16)
    nhalf = sbt(top, "nhalf", [128, 4], F32)
    a1_col = sbt(top, "a1_col", [128, 8], F32)
    b1_col = sbt(top, "b1_col", [128, 8], F32)
    g1_bc = sbt(top, "g1_bc", [128, D], F32)
    a2_bc = sbt(top, "a2_bc", [128, D], F32)
    b2_bc = sbt(top, "b2_bc", [128, D], F32)
    g2_bc = sbt(top, "g2_bc", [128, D], F32)
    fing_sb = sbt(top, "fing_sb", [128, D], F32)
    kmT_sb = sbt(top, "kmT_sb", [128, 8, 32], F32)
    nkm_bc = sbt(top, "nkm_bc", [128, 16], F32)
    nlam = sbt(top, "nlam", [128, 1], F32)
    subg_sb = sbt(top, "subg_sb", [128, 128], F32)
    wr_sb = sbt(top, "wr_sb", [128, 8, 36], F32)
    br_sb = sbt(top, "br_sb", [128, 36], F32)
    ebase_sb = sbt(top, "ebase_sb", [128, 32], F32)
    pos_i = sbt(top, "pos_i", [128, NT_OWN, 2], I32)
    wts_sb = sbt(top, "wts_sb", [128, NT_OWN, 2], F32)
    carry = sbt(top, "carry", [128, 32], F32)

    with ExitStack() as es:
        c_sb = sbt(es, "c_sb", [128, 8], F32)
        cact = sbt(es, "cact", [128, 8], F32)
        cact_rep = sbt(es, "cact_rep", [128, 8, 128], F32)
        bada_sb = sbt(es, "bada_sb", [128, 6 * D], F32)
        mod_bc = sbt(es, "mod_bc", [128, 6 * D], F32)
        n1g_sb = sbt(es, "n1g_sb", [128, D], F32)
        n2g_sb = sbt(es, "n2g_sb", [128, D], F32)
        utri_f = sbt(es, "utri_f", [128, 128], F32)
        lam_sb = sbt(es, "lam_sb", [128, 4, 64], F32)
        lamt = sbt(es, "lamt", [128, 8], F32)
        tmpD = sbt(es, "tmpD", [128, D], F32)
        tmpE = sbt(es, "tmpE", [128, D], F32)
        zer = sbt(es, "zer", [128, 4, D], BF16)
        wa = [sbt(es, "wa%d" % i, [128, 8, 512], F32) for i in range(2)]
        pm = [pst(es, "pm%d" % i, [128, 512], F32) for i in range(2)]
        Bc, Bca, Bcr, Bbada, Bmod, Bn1, Bn2, Butf, Blam, Blt, BtD, BtE, Bzer = [Buf(n) for n in
            ("c", "cact", "cactrep", "bada", "mod", "n1g", "n2g", "utf", "lam", "lamt", "tmpD", "tmpE", "zer")]
        Bwa = [Buf("wa0"), Buf("wa1")]
        Bpm = [Buf("pm0"), Buf("pm1")]
        Bconst = Buf("const")
        Bpers = Buf("pers")

        for (dst, src) in ((ident_f[:], ident_h), (fing_sb[:], fing_bc), (subg_sb[:], subg_bc),
                           (br_sb[:], br_bc), (ebase_sb[:], ebase_bc),
                           (wr_sb[:], w_r.rearrange("(k p) n -> p k n", p=128))):
            p.dma("sp", DMA(dst, src), writes=[Bconst], waw=False)
        p.dma("sp", DMA(c_sb[:], c_col), writes=[Bc])
        p.dma("sp", DMA(bada_sb[:], bada_bc), writes=[Bbada])
        p.dma("sp", DMA(n1g_sb[:], n1g_bc), writes=[Bn1])
        p.dma("sp", DMA(n2g_sb[:], n2g_bc), writes=[Bn2])
        p.dma("sp", DMA(utri_f[:], utri_h), writes=[Butf])
        p.dma("sp", DMA(lam_sb[:], lam4), writes=[Blam])
        p.op("dve", MSET(ones_f[:], 1.0), writes=[Bpers])
        p.op("dve", MSET(ones_b[:], 1.0), writes=[Bpers])
        p.op("dve", MSET(nhalf[:], -0.5), writes=[Bpers])
        p.op("dve", MSET(carry[:], 0.0), writes=[Bpers])
        p.op("dve", MSET(zer[:], 0.0), writes=[Bzer])
        p.op("dve", CPY(ident_b[:], ident_f[:]), reads=[Bconst], writes=[Bpers])
        p.op("dve", CPY(utri_b[:], utri_f[:]), reads=[Butf], writes=[Bpers])
        p.op("dve", TS(subg_sb[:], subg_sb[:], 0.8, None, ALU.mult), reads=[Bconst], writes=[Bpers])
        p.op("dve", TT(lam_sb[:, 0, :], lam_sb[:, 0, :], lam_sb[:, 1, :], ALU.mult), reads=[Blam], writes=[Blam])
        p.op("dve", TT(lam_sb[:, 2, :], lam_sb[:, 2, :], lam_sb[:, 3, :], ALU.mult), reads=[Blam], writes=[Blam])
        p.op("dve", RED(lamt[:, 0:1], lam_sb[:, 0, :], ALU.add), reads=[Blam], writes=[Blt])
        p.op("dve", RED(lamt[:, 1:2], lam_sb[:, 2, :], ALU.add), reads=[Blam], writes=[Blt])
        p.op("act", ACTF(lamt[:, 2:4], lamt[:, 0:2], AF.Exp), reads=[Blt], writes=[Blt])
        p.op("dve", TT(lamt[:, 4:5], lamt[:, 3:4], lamt[:, 2:3], ALU.subtract), reads=[Blt], writes=[Blt])
        p.op("dve", TS(nlam[:], lamt[:, 4:5], -0.2, None, ALU.add), reads=[Blt], writes=[Bpers])
        p.op("act", ACTF(cact[:], c_sb[:], AF.Sigmoid), reads=[Bc], writes=[Bca])
        p.op("dve", TT(cact[:], cact[:], c_sb[:], ALU.mult), reads=[Bc, Bca], writes=[Bca])
        p.op("dve", CPY(cact_rep[:], bc(cact[:].unsqueeze(2), [128, 8, 128])), reads=[Bca], writes=[Bcr])
        for cg in range(12):
            sl = slice(cg * 512, (cg + 1) * 512)
            p.dma("sp", DMA(wa[cg % 2][:], w_ada[:, sl].rearrange("(k p) n -> p k n", p=128)), writes=[Bwa[cg % 2]])
            p.op("pe", MMS([(pm[cg % 2][:], cact_rep[:, k, :], wa[cg % 2][:, k, :], k == 0, k == 7) for k in range(8)]),
                 reads=[Bcr, Bwa[cg % 2]], writes=[Bpm[cg % 2]])
            p.op("dve", TT(mod_bc[:, sl], pm[cg % 2][:], bada_sb[:, sl], ALU.add),
                 reads=[Bpm[cg % 2], Bbada], writes=[Bmod])
        p.op("dve", STT(tmpD[:], mod_bc[:, D:2 * D], 1.0, n1g_sb[:], ALU.add, ALU.mult), reads=[Bmod, Bn1], writes=[BtD])
        p.op("dve", TT(tmpE[:].rearrange("p (k j) -> p k j", j=128), tmpD[:].rearrange("p (k j) -> p k j", j=128),
                       bc(ident_f[:].unsqueeze(1), [128, 8, 128]), ALU.mult), reads=[BtD, Bconst], writes=[BtE])
        p.op("dve", RED(a1_col[:], tmpE[:].rearrange("p (k j) -> p k j", j=128), ALU.add), reads=[BtE], writes=[Bpers])
        p.op("dve", TT(tmpE[:].rearrange("p (k j) -> p k j", j=128), mod_bc[:, 0:D].rearrange("p (k j) -> p k j", j=128),
                       bc(ident_f[:].unsqueeze(1), [128, 8, 128]), ALU.mult), reads=[Bmod, Bconst], writes=[BtE])
        p.op("dve", RED(b1_col[:], tmpE[:].rearrange("p (k j) -> p k j", j=128), ALU.add), reads=[BtE], writes=[Bpers])
        p.op("dve", CPY(g1_bc[:], mod_bc[:, 2 * D:3 * D]), reads=[Bmod], writes=[Bpers])
        p.op("dve", CPY(b2_bc[:], mod_bc[:, 3 * D:4 * D]), reads=[Bmod], writes=[Bpers])
        p.op("dve", STT(a2_bc[:], mod_bc[:, 4 * D:5 * D], 1.0, n2g_sb[:], ALU.add, ALU.mult), reads=[Bmod, Bn2], writes=[Bpers])
        p.op("dve", CPY(g2_bc[:], mod_bc[:, 5 * D:6 * D]), reads=[Bmod], writes=[Bpers])
        p.barrier()
        p.emit()

    if stage < 1:
        return finish(nc, p, top, out_h, BOUT)

    def norm_part(x_t, Bx, ss, Bss, sqj, Bsqj, xn, Bxn):
        p.op("act", ACTF(sqj[:], x_t[:], AF.Square, accum=ss[:, 0:1]), reads=[Bx], writes=[Bsqj, Bss])
        p.op("dve", TS(ss[:, 1:2], ss[:, 0:1], 1.0 / D, EPS, ALU.mult, ALU.add), reads=[Bss], writes=[Bss])
        p.op("pool", TT(ss[:, 2:3], ss[:, 1:2], nhalf[:, 0:1], ALU.pow), reads=[Bss], writes=[Bss])
        p.op("dve", TS(xn[:], x_t[:], ss[:, 2:3], None, ALU.mult), reads=[Bx, Bss], writes=[Bxn])

    def transpose_part(xn, Bxn, psT, BpsT, hT, BhT):
        p.op("pe", TRS([(psT[:, k * 128:(k + 1) * 128], xn[:, k * 128:(k + 1) * 128], ident_b[:]) for k in range(8)]),
             reads=[Bxn], writes=[BpsT])
        for k in range(8):
            if k % 4 == 0:
                p.op("dve", TS(hT[:, k, :], psT[:, k * 128:(k + 1) * 128], a1_col[:, k:k + 1], b1_col[:, k:k + 1],
                               ALU.mult, ALU.add), reads=[BpsT], writes=[BhT])
            else:
                p.op("act", ACTF(hT[:, k, :], psT[:, k * 128:(k + 1) * 128], AF.Identity,
                                 bias=b1_col[:, k:k + 1], scale=a1_col[:, k:k + 1]), reads=[BpsT], writes=[BhT])

    def pipeline(nt, stages):
        ns = len(stages)
        for s_ in range(nt + ns - 1):
            for k_, f_ in enumerate(stages):
                t_ = s_ - k_
                if 0 <= t_ < nt:
                    f_(t_)

    def rope(src, dst, cs_t, Bsrc, Bdst, t1, t2, Bt1, Bt2, nh):
        sv = src.rearrange("p (h two d) -> p h two d", two=2, d=32)
        dv = dst.rearrange("p (h two d) -> p h two d", two=2, d=32)
        x1, x2 = sv[:, :, 0, :], sv[:, :, 1, :]
        cosb = bc(cs_t[:, 0:32].unsqueeze(1), [128, nh, 32])
        sinb = bc(cs_t[:, 32:64].unsqueeze(1), [128, nh, 32])
        a = t1.rearrange("p (h d) -> p h d", d=32)
        b = t2.rearrange("p (h d) -> p h d", d=32)
        p.op("dve", TT(a, x1, cosb, ALU.mult), reads=[Bsrc], writes=[Bt1])
        p.op("dve", TT(b, x2, sinb, ALU.mult), reads=[Bsrc], writes=[Bt2])
        p.op("dve", TT(dv[:, :, 0, :], a, b, ALU.subtract), reads=[Bt1, Bt2], writes=[Bdst])
        p.op("dve", TT(a, x2, cosb, ALU.mult), reads=[Bsrc, Bdst], writes=[Bt1])
        p.op("dve", TT(b, x1, sinb, ALU.mult), reads=[Bsrc, Bdst], writes=[Bt2])
        p.op("dve", TT(dv[:, :, 1, :], a, b, ALU.add), reads=[Bt1, Bt2], writes=[Bdst])

    with ExitStack() as es:
        wkv = sbt(es, "wkv", [128, 8, 2048], BF16)
        csA = sbt(es, "csA", [128, NT_ALL, 64], F32)
        xt = [sbt(es, "xt%d" % i, [128, D], F32) for i in range(3)]
        ss = [sbt(es, "ss%d" % i, [128, 4], F32) for i in range(2)]
        sqj = sbt(es, "sqj", [128, D], BF16)
        xn = [sbt(es, "xn%d" % i, [128, D], BF16) for i in range(2)]
        hT = [sbt(es, "hT%d" % i, [128, 8, 128], BF16) for i in range(2)]
        kraw = [sbt(es, "kraw%d" % i, [128, D], F32) for i in range(2)]
        kr = [sbt(es, "kr%d" % i, [128, D], F32) for i in range(2)]
        t1 = sbt(es, "t1", [128, 512], F32)
        t2 = sbt(es, "t2", [128, 512], F32)
        sq = sbt(es, "sq", [128, D], F32)
        kss = sbt(es, "kss", [128, 16], F32)
        kmaxacc = sbt(es, "kmaxacc", [128, 16], F32)
        kb = [sbt(es, "kb%d" % i, [128, D], BF16) for i in range(2)]
        vst = [sbt(es, "vst%d" % i, [128, D], BF16) for i in range(2)]
        ktst = [sbt(es, "ktst%d" % i, [128, 8, 512], BF16) for i in range(2)]
        kmx = sbt(es, "kmx", [16, 4], F32)
        dg = sbt(es, "dg", [16, 16], F32)
        psT = pst(es, "psT", [128, D], BF16)
        pp = pst(es, "pp", [128, 2048], F32)
        pkt = pst(es, "pkt", [128, D], BF16)
        pkm = pst(es, "pkm", [128, 128], F32)
        pmisc = pst(es, "pmisc", [128, 128], F32)
        Bwkv, BcsA, Bsqj, Bt1, Bt2, Bsq, Bkss, Bkma, BpsT, Bpp, Bpkt, Bpkm, Bpmisc, Bkmx, Bdg = [Buf(n) for n in
            ("wkv", "csA", "sqj", "t1", "t2", "sq", "kss", "kma", "psT", "pp", "pkt", "pkm", "pmisc", "kmx", "dg")]
        Bxt = [Buf("xt%d" % i) for i in range(3)]
        Bss = [Buf("ss%d" % i) for i in range(2)]
        Bxn = [Buf("xn%d" % i) for i in range(2)]
        BhT = [Buf("hT%d" % i) for i in range(2)]
        Bkraw = [Buf("kraw%d" % i) for i in range(2)]
        Bkr = [Buf("kr%d" % i) for i in range(2)]
        Bkb = [Buf("kb%d" % i) for i in range(2)]
        Bvst = [Buf("vst%d" % i) for i in range(2)]
        Bktst = [Buf("ktst%d" % i) for i in range(2)]

        segs = [(512, 1024), (1024, 1536), (2048, 2560), (2560, 3072)]
        for gi, (a, b) in enumerate(segs):
            p.dma("pool", DMA(wkv[:, :, gi * 512:(gi + 1) * 512], w_in[:, a:b].rearrange("(k p) n -> p k n", p=128)),
                  writes=[Bwkv], waw=False)
        p.dma("sp", DMA(csA[:], cs_all), writes=[BcsA])
        p.op("dve", MSET(kmaxacc[:], 0.0), writes=[Bkma])
        Bppa, Bppb = Buf("ppa"), Buf("ppb")

        def a0(t):
            p.dma("sp", DMA(xt[t % 3][:], xall[t * 128:(t + 1) * 128, :]), writes=[Bxt[t % 3]])

        def a1(t):
            u = t % 2
            norm_part(xt[t % 3], Bxt[t % 3], ss[u], Bss[u], sqj, Bsqj, xn[u], Bxn[u])

        def a2(t):
            u = t % 2
            transpose_part(xn[u], Bxn[u], psT, BpsT, hT[u], BhT[u])

        def bstage(t):
            u = t % 2
            p.op("pe", MMS([(pp[:, g * 512:(g + 1) * 512], hT[u][:, k, :], wkv[:, k, g * 512:(g + 1) * 512], k == 0, k == 7)
                            for k in range(8) for g in (0, 2)]), reads=[BhT[u], Bwkv], writes=[Bppa])
            p.op("pe", MMS([(pp[:, g * 512:(g + 1) * 512], hT[u][:, k, :], wkv[:, k, g * 512:(g + 1) * 512], k == 0, k == 7)
                            for k in range(8) for g in (1, 3)]), reads=[BhT[u], Bwkv], writes=[Bppb])
            p.op("act", ACPY(kraw[u][:, 0:512], pp[:, 0:512]), reads=[Bppa], writes=[Bkraw[u]])
            p.op("act", ACPY(kraw[u][:, 512:1024], pp[:, 1024:1536]), reads=[Bppa], writes=[Bkraw[u]])
            p.op("act", ACPY(vst[u][:, 0:512], pp[:, 512:1024]), reads=[Bppb], writes=[Bvst[u]])
            p.op("dve", CPY(vst[u][:, 512:1024], pp[:, 1536:2048]), reads=[Bppb], writes=[Bvst[u]])
            p.dma("act", DMA(VS[t * 128:(t + 1) * 128, :], vst[u][:]), reads=[Bvst[u]], writes=[BVS], waw=False)

        def cstage(t):
            u = t % 2
            rope(kraw[u][:], kr[u][:], csA[:, t, :], Bkraw[u], Bkr[u], t1[:], t2[:], Bt1, Bt2, 16)
            p.op("pool", TT(sq[:], kr[u][:], kr[u][:], ALU.mult), reads=[Bkr[u]], writes=[Bsq])
            p.op("dve", RED(kss[:], sq[:].rearrange("p (h d) -> p h d", d=64), ALU.add), reads=[Bsq], writes=[Bkss])
            p.op("dve", TT(kmaxacc[:], kmaxacc[:], kss[:], ALU.max), reads=[Bkss], writes=[Bkma])
            p.op("act", ACPY(kb[u][:], kr[u][:]), reads=[Bkr[u]], writes=[Bkb[u]])
            n = t // 2
            p.op("pe", MMS([(pkm[:, j * 32 + n:j * 32 + n + 1], kr[u][:, j * 128:(j + 1) * 128], ones_f[:, 0:1],
                             t % 2 == 0 and j == 0, t % 2 == 1) for j in range(4)]), reads=[Bkr[u]], writes=[Bpkm])
            p.op("pe", TRS([(pkt[:, j * 128:(j + 1) * 128], kb[u][:, j * 128:(j + 1) * 128], ident_b[:]) for j in range(8)]),
                 reads=[Bkb[u]], writes=[Bpkt])
            g, tt = t // 4, t % 4
            p.op("dve", CPY(ktst[g % 2][:, :, tt * 128:(tt + 1) * 128], pkt[:].rearrange("p (j n) -> p j n", n=128)),
                 reads=[Bpkt], writes=[Bktst[g % 2]])
            if tt == 3:
                p.dma("act", DMA(KT2[:, :, g * 512:(g + 1) * 512].rearrange("j p s -> p j s"), ktst[g % 2][:]),
                      reads=[Bktst[g % 2]], writes=[BKT], waw=False)

        pipeline(NT_ALL, [a0, a1, a2, bstage, cstage])
        p.op("dve", MSET(kmT_sb[:], 0.0), writes=[Bpers])
        pkv = pkm[:].rearrange("p (j n) -> p j n", n=32)
        kmv = kmT_sb[:].rearrange("p (j two) n -> p j two n", two=2)
        p.op("dve", CPY(kmv[0:64, :, 0, :], pkv[0:64, :, :]), reads=[Bpkm], writes=[Bpers])
        p.op("dve", CPY(kmv[64:128, :, 1, :], pkv[64:128, :, :]), reads=[Bpkm], writes=[Bpers])
        p.op("pe", TRS([(pmisc[0:16, 0:128], kmaxacc[:, 0:16], ident_f[:])]), reads=[Bkma], writes=[Bpmisc])
        p.op("dve", RED(kmx[:, 0:1], pmisc[0:16, 0:128], ALU.max), reads=[Bpmisc], writes=[Bkmx])
        p.op("dve", TS(dg[:], ident_f[0:16, 0:16], kmx[:, 0:1], -0.0625, ALU.mult, ALU.mult), reads=[Bkmx], writes=[Bdg])
        p.op("pe", MM(pmisc[:, 0:16], ones_f[0:16, 0:128], dg[:]), reads=[Bdg, Bkmx], writes=[Bpmisc])
        p.op("dve", CPY(nkm_bc[:], pmisc[:, 0:16]), reads=[Bpmisc], writes=[Bpers])
        p.barrier()
        p.emit()

    if stage < 2:
        return finish(nc, p, top, out_h, BOUT)

    with ExitStack() as es:
        wq = sbt(es, "wq", [128, 8, 1024], BF16)
        csO = sbt(es, "csO", [128, NT_OWN, 64], F32)
        bval = sbt(es, "bval", [128, NT_OWN, 32], F32)
        oblk = sbt(es, "oblk", [128, NT_OWN, 32], F32)
        xt = [sbt(es, "xt%d" % i, [128, D], F32) for i in range(3)]
        ss = [sbt(es, "ss%d" % i, [128, 4], F32) for i in range(2)]
        sqj = sbt(es, "sqj", [128, D], BF16)
        xn = [sbt(es, "xn%d" % i, [128, D], BF16) for i in range(2)]
        hst = [sbt(es, "hst%d" % i, [128, 8, 512], BF16) for i in range(2)]
        qraw = [sbt(es, "qraw%d" % i, [128, D], F32) for i in range(2)]
        qr = [sbt(es, "qr%d" % i, [128, D], F32) for i in range(2)]
        t1 = sbt(es, "t1", [128, 512], F32)
        t2 = sbt(es, "t2", [128, 512], F32)
        sq = sbt(es, "sq", [128, D], F32)
        qss = sbt(es, "qss", [128, 16], F32)
        A = [sbt(es, "A%d" % i, [128, 16, 128], BF16) for i in range(2)]
        qT = sbt(es, "qT", [128, 4, 128], F32)
        gm2 = [sbt(es, "gm%d" % i, [128, 8, 32], F32) for i in range(2)]
        Bgm2 = [Buf("gm0"), Buf("gm1")]
        top8 = sbt(es, "top8", [128, 8, 8], F32)
        sel = sbt(es, "sel", [128, 8, 32], F32)
        val = sbt(es, "val", [128, 8, 32], F32)
        qtst = [sbt(es, "qtst%d" % i, [128, 16, 512], BF16) for i in range(2)]
        psT = pst(es, "psT", [128, D], BF16)
        pq = pst(es, "pq", [128, D], F32)
        pqT = pst(es, "pqT", [128, 512], F32)
        pg = pst(es, "pg", [128, 256], F32)
        pA = pst(es, "pA", [128, 2048], BF16)
        Bwq, BcsO, Bbv, Bob, Bsqj, Bt1, Bt2, Bsq, Bqss, BqT, Bgm, Btop8, Bsel, Bval, BpsT, Bpq, BpqT, Bpg, BpA = [
            Buf(n) for n in ("wq", "csO", "bv", "ob", "sqj", "t1", "t2", "sq", "qss", "qT", "gm", "top8", "sel", "val",
                             "psT", "pq", "pqT", "pg", "pA")]
        Bxt = [Buf("xt%d" % i) for i in range(3)]
        Bss = [Buf("ss%d" % i) for i in range(2)]
        Bxn = [Buf("xn%d" % i) for i in range(2)]
        Bhst = [Buf("hst%d" % i) for i in range(2)]
        Bqraw = [Buf("qraw%d" % i) for i in range(2)]
        Bqr = [Buf("qr%d" % i) for i in range(2)]
        BA = [Buf("A%d" % i) for i in range(2)]
        Bqtst = [Buf("qtst%d" % i) for i in range(2)]

        for gi, (a, b) in enumerate([(0, 512), (1536, 2048)]):
            p.dma("pool", DMA(wq[:, :, gi * 512:(gi + 1) * 512], w_in[:, a:b].rearrange("(k p) n -> p k n", p=128)),
                  writes=[Bwq], waw=False)
        p.dma("sp", DMA(csO[:], cs_own), writes=[BcsO])
        p.dma("sp", DMA(bval[:], blkvalid), writes=[Bbv])
        p.dma("sp", DMA(oblk[:], ownblk), writes=[Bob])
        for i in range(2):
            p.op("dve", MSET(A[i][:], 0.0), writes=[BA[i]])
        def a0(t):
            p.dma("sp", DMA(xt[t % 3][:], xown[t * 128:(t + 1) * 128, :]), writes=[Bxt[t % 3]])

        def a1(t):
            u = t % 2
            norm_part(xt[t % 3], Bxt[t % 3], ss[u], Bss[u], sqj, Bsqj, xn[u], Bxn[u])

        class _H:
            def __init__(self, v):
                self.v = v

            def __getitem__(self, key):
                return self.v[key]

        def a2(t):
            u = t % 2
            g, tt = t // 4, t % 4
            hTv = hst[g % 2][:, :, tt * 128:(tt + 1) * 128]
            transpose_part(xn[u], Bxn[u], psT, BpsT, _H(hTv), Bhst[g % 2])

        def bstage(t):
            u = t % 2
            g, tt = t // 4, t % 4
            hTv = hst[g % 2][:, :, tt * 128:(tt + 1) * 128]
            p.op("pe", MMS([(pq[:, gq * 512:(gq + 1) * 512], hTv[:, k, :], wq[:, k, gq * 512:(gq + 1) * 512], k == 0, k == 7)
                            for k in range(8) for gq in range(2)]), reads=[Bhst[g % 2], Bwq], writes=[Bpq])
            if tt == 3:
                p.dma("act", DMA(HT[:, :, g * 512:(g + 1) * 512], hst[g % 2][:]), reads=[Bhst[g % 2]], writes=[BHT], waw=False)
            p.op("act", ACPY(qraw[u][:, 0:512], pq[:, 0:512]), reads=[Bpq], writes=[Bqraw[u]])
            p.op("act", ACPY(qraw[u][:, 512:1024], pq[:, 512:1024]), reads=[Bpq], writes=[Bqraw[u]])

        def c1(t):
            u = t % 2
            rope(qraw[u][:], qr[u][:], csO[:, t, :], Bqraw[u], Bqr[u], t1[:], t2[:], Bt1, Bt2, 16)
            p.op("pool", TT(sq[:], qr[u][:], qr[u][:], ALU.mult), reads=[Bqr[u]], writes=[Bsq])
            p.op("dve", RED(qss[:], sq[:].rearrange("p (h d) -> p h d", d=64), ALU.add), reads=[Bsq], writes=[Bqss])
            Au, BAu = A[u], BA[u]
            p.op("dve", STT(Au[:, :, 96], qss[:], -0.0625, nkm_bc[:], ALU.mult, ALU.add), reads=[Bqss], writes=[BAu])
            p.op("act", ACTF(Au[:, :, 0:64], qr[u][:].rearrange("p (h d) -> p h d", d=64), AF.Copy, scale=0.125),
                 reads=[Bqr[u]], writes=[BAu])
            p.op("pe", TRS([(pqT[:, j * 128:(j + 1) * 128], qr[u][:, j * 128:(j + 1) * 128], ident_f[:]) for j in range(4)]),
                 reads=[Bqr[u]], writes=[BpqT])
            p.op("act", ACPY(qT[:], pqT[:].rearrange("p (j n) -> p j n", n=128)), reads=[BpqT], writes=[BqT])
            p.op("pe", MMS([(pg[:, h * 32:(h + 1) * 32], qT[:, h // 2, :], kmT_sb[:, h, :], True, True) for h in range(8)]),
                 reads=[BqT], writes=[Bpg])
            p.op("dve", TT(gm2[u][:], pg[:].rearrange("p (h n) -> p h n", n=32), bc(bval[:, t, :].unsqueeze(1), [128, 8, 32]),
                           ALU.add), reads=[Bpg, Bbv], writes=[Bgm2[u]])

        def c2(t):
            u = t % 2
            g, tt = t // 4, t % 4
            Au, BAu = A[u], BA[u]
            gm, Bgm = gm2[u], Bgm2[u]
            for h in range(8):
                p.op("dve", (lambda o, i: (lambda e: e.max(out=o, in_=i)))(top8[:, h, :], gm[:, h, :]),
                     reads=[Bgm], writes=[Btop8])
            p.op("dve", TT(sel[:], gm[:], bc(top8[:, :, 2:3], [128, 8, 32]), ALU.is_ge), reads=[Bgm, Btop8], writes=[Bsel])
            p.op("dve", TS(val[:], gm[:], -1e29, None, ALU.is_gt), reads=[Bgm], writes=[Bval])
            p.op("dve", TT(sel[:], sel[:], val[:], ALU.mult), reads=[Bval], writes=[Bsel])
            p.op("dve", TT(sel[:], sel[:], bc(oblk[:, t, :].unsqueeze(1), [128, 8, 32]), ALU.max), reads=[Bob], writes=[Bsel])
            p.op("dve", TS(Au[:, 0:8, 64:96], sel[:], 30000.0, -30000.0, ALU.mult, ALU.add), reads=[Bsel], writes=[BAu])
            p.op("pe", TRS([(pA[:, h * 128:(h + 1) * 128], Au[:, h, :], ident_b[:]) for h in range(16)]),
                 reads=[BAu], writes=[BpA])
            pAv = pA[:].rearrange("p (h n) -> p h n", n=128)
            p.op("dve", CPY(qtst[g % 2][:, 0:8, tt * 128:(tt + 1) * 128], pAv[:, 0:8, :]), reads=[BpA], writes=[Bqtst[g % 2]])
            p.op("act", ACPY(qtst[g % 2][:, 8:16, tt * 128:(tt + 1) * 128], pAv[:, 8:16, :]), reads=[BpA], writes=[Bqtst[g % 2]])
            if tt == 3:
                p.dma("act", DMA(QT[:, :, g * 512:(g + 1) * 512], qtst[g % 2][:]), reads=[Bqtst[g % 2]], writes=[BQT], waw=False)

        pipeline(NT_OWN, [a0, a1, a2, bstage, c1, c2])
        p.barrier()
        p.emit()

    if stage < 3:
        return finish(nc, p, top, out_h, BOUT)

    with ExitStack() as es:
        KTs = [sbt(es, "KTs%d" % i, [128, S], BF16) for i in range(2)]
        QTs = [sbt(es, "QTs%d" % i, [128, NO], BF16) for i in range(2)]
        Vm = [sbt(es, "Vm%d" % i, [128, NT_ALL, 65], BF16) for i in range(2)]
        Vd = [sbt(es, "Vd%d" % i, [128, NT_ALL, 129], BF16) for i in range(2)]
        cm = sbt(es, "cm", [128, 16, 512], BF16)
        PT = [sbt(es, "PT%d" % i, [128, 1024], BF16) for i in range(3)]
        o1s = sbt(es, "o1s", [128, NT_OWN, 128], F32)
        rinv = sbt(es, "rinv", [128, 4], F32)
        omst = [sbt(es, "omst%d" % i, [128, 4, 64], BF16) for i in range(2)]
        odst = [sbt(es, "odst%d" % i, [128, 4, 128], BF16) for i in range(2)]
        o2t = sbt(es, "o2t", [128, 4, 128], F32)
        odt = sbt(es, "odt", [128, 4, 128], F32)
        sq4 = sbt(es, "sq4", [128, 4, 128], F32)
        st4 = sbt(es, "st4", [128, 8], F32)
        pS = [pst(es, "pS%d" % i, [128, 1024], F32) for i in range(2)]
        pO = [pst(es, "pO%d" % i, [128, 1024], F32) for i in range(2)]
        BKTs = [Buf("KTs%d" % i) for i in range(2)]
        BQTs = [Buf("QTs%d" % i) for i in range(2)]
        BVm = [Buf("Vm%d" % i) for i in range(2)]
        BVd = [Buf("Vd%d" % i) for i in range(2)]
        BPT = [Buf("PT%d" % i) for i in range(3)]
        BpS = [Buf("pS%d" % i) for i in range(2)]
        BpO = [Buf("pO%d" % i) for i in range(2)]
        Bcm, Bo1s, Brinv, Bo2t, Bodt, Bsq4, Bst4 = [Buf(n) for n in ("cm", "o1s", "rinv", "o2t", "odt", "sq4", "st4")]
        Bomst = [Buf("omst%d" % i) for i in range(2)]
        Bodst = [Buf("odst%d" % i) for i in range(2)]

        p.dma("pool", DMA(cm[:], cmask), writes=[Bcm])
        for i in range(2):
            p.dma("pool", DMA(KTs[i][64:97, :], khot), writes=[BKTs[i]])
            p.op("dve", MSET(Vm[i][:, :, 64:65], 1.0), writes=[BVm[i]])
            p.op("dve", MSET(Vd[i][:, :, 128:129], 1.0), writes=[BVd[i]])
        VSv = VS.rearrange("(t p) c -> p t c", p=128)
        OMv = OM.rearrange("(n p) c -> p n c", p=128)
        ODv = OD.rearrange("(n p) c -> p n c", p=128)

        def load_head(hi):
            u = hi % 2
            p.dma("sp", DMA(KTs[u][0:64, :], KT2[hi // 2, (hi % 2) * 64:(hi % 2) * 64 + 64, :]), reads=[BKT], writes=[BKTs[u]])
            p.dma("sp", DMA(QTs[u][:], QT[:, hi, :]), reads=[BQT], writes=[BQTs[u]])
            if hi < 8:
                p.dma("sp", DMA(Vm[u][:, :, 0:64], VSv[:, :, hi * 64:(hi + 1) * 64]), reads=[BVS], writes=[BVm[u]])
            elif (hi - 8) % 2 == 0:
                hd = (hi - 8) // 2
                p.dma("sp", DMA(Vd[hd % 2][:, :, 0:128], VSv[:, :, 512 + hd * 128:512 + (hd + 1) * 128]),
                      reads=[BVS], writes=[BVd[hd % 2]])

        head_list = list(range(16))
        items = []
        for hn, hi in enumerate(head_list):
            for i in range(8):
                for pr in range(4 * (i + 1)):
                    items.append((hn, hi, i, pr))
        N = len(items)
        oidx_of = {}
        for (hn, hi, i, pr) in items:
            if (hn, i) not in oidx_of:
                oidx_of[(hn, i)] = len(oidx_of)

        def emit_qk(n):
            hn, hi, i, pr = items[n]
            u = hi % 2
            ps_, Bps = pS[n % 2], BpS[n % 2]
            kt0 = 2 * pr
            p.op("pe", MMS([(ps_[:, kk * 512:(kk + 1) * 512], KTs[u][0:97, (kt0 + kk) * 128:(kt0 + kk + 1) * 128],
                             QTs[u][0:97, i * 512:(i + 1) * 512], True, True) for kk in range(2)]),
                 reads=[BKTs[u], BQTs[u]], writes=[Bps])

        def emit_exp(n):
            hn, hi, i, pr = items[n]
            ps_, Bps = pS[n % 2], BpS[n % 2]
            pt, Bpt = PT[n % 3], BPT[n % 3]
            nkt = 8 * (i + 1)
            kt0 = 2 * pr
            p.op("act", ACTF(pt[:], ps_[:], AF.Exp), reads=[Bps], writes=[Bpt])
            if kt0 >= nkt - 8:
                m0 = (i % 2) * 8 + (kt0 - (nkt - 8))
                p.op("dve", TT(pt[:], pt[:], cm[:, m0:m0 + 2, :].rearrange("p a q -> p (a q)"), ALU.mult),
                     reads=[Bcm], writes=[Bpt])

        def emit_av(n):
            hn, hi, i, pr = items[n]
            u = hi % 2
            moba = hi < 8
            if i == 0 and pr == 0 and hn + 1 < len(head_list):
                load_head(head_list[hn + 1])
            if moba:
                Vt, BV, W = Vm[u], BVm[u], 65
            else:
                hd, cc = (hi - 8) // 2, (hi - 8) % 2
                Vt, BV, W = Vd[hd % 2], BVd[hd % 2], 129
            npairs = 4 * (i + 1)
            oi = oidx_of[(hn, i)]
            po, Bpo = pO[oi % 2], BpO[oi % 2]
            pt, Bpt = PT[n % 3], BPT[n % 3]
            kt0 = 2 * pr
            p.op("pe", MMS([(po[:, qs * 256:qs * 256 + W], pt[:, kk * 512 + qs * 128:kk * 512 + (qs + 1) * 128],
                             Vt[:, kt0 + kk, 0:W], pr == 0 and kk == 0 and qs % 2 == 0, pr == npairs - 1 and kk == 1)
                            for kk in range(2) for qs in range(4)]),
                 reads=[Bpt, BV], writes=[Bpo])
            if pr != npairs - 1:
                return
            pov = po[:].rearrange("p (q w) -> p q w", w=256)
            if moba:
                p.op("dve", (lambda o, i_: (lambda e: e.reciprocal(out=o, in_=i_)))(rinv[:], pov[:, :, 64]),
                     reads=[Bpo], writes=[Brinv])
                om, Bom = omst[i % 2], Bomst[i % 2]
                p.op("dve", TT(om[:], pov[:, :, 0:64], bc(rinv[:].unsqueeze(2), [128, 4, 64]), ALU.mult),
                     reads=[Bpo, Brinv], writes=[Bom])
                p.dma("pool", DMA(OMv[:, i * 4:(i + 1) * 4, hi * 64:(hi + 1) * 64], om[:]), reads=[Bom], writes=[BOM], waw=False)
            else:
                p.op("dve", (lambda o, i_: (lambda e: e.reciprocal(out=o, in_=i_)))(rinv[:], pov[:, :, 128]),
                     reads=[Bpo], writes=[Brinv])
                if cc == 0:
                    p.op("dve", TT(o1s[:, i * 4:(i + 1) * 4, :], pov[:, :, 0:128], bc(rinv[:].unsqueeze(2), [128, 4, 128]),
                                   ALU.mult), reads=[Bpo, Brinv], writes=[Bo1s])
                else:
                    p.op("dve", TT(o2t[:], pov[:, :, 0:128], bc(rinv[:].unsqueeze(2), [128, 4, 128]), ALU.mult),
                         reads=[Bpo, Brinv], writes=[Bo2t])
                    p.op("dve", STT(odt[:], o2t[:], nlam[:, 0:1], o1s[:, i * 4:(i + 1) * 4, :], ALU.mult, ALU.add),
                         reads=[Bo2t, Bo1s], writes=[Bodt])
                    p.op("pool", TT(sq4[:], odt[:], odt[:], ALU.mult), reads=[Bodt], writes=[Bsq4])
                    p.op("dve", RED(st4[:, 0:4], sq4[:], ALU.add), reads=[Bsq4], writes=[Bst4])
                    p.op("dve", TS(st4[:, 0:4], st4[:, 0:4], 1.0 / 128, EPS, ALU.mult, ALU.add), reads=[Bst4], writes=[Bst4])
                    p.op("pool", TT(st4[:, 4:8], st4[:, 0:4], nhalf[:, 0:4], ALU.pow), reads=[Bst4], writes=[Bst4])
                    p.op("dve", TT(odt[:], odt[:], bc(st4[:, 4:8].unsqueeze(2), [128, 4, 128]), ALU.mult),
                         reads=[Bst4], writes=[Bodt])
                    od_, Bod_ = odst[i % 2], Bodst[i % 2]
                    p.op("dve", TT(od_[:], odt[:], bc(subg_sb[:].unsqueeze(1), [128, 4, 128]), ALU.mult),
                         reads=[Bodt], writes=[Bod_])
                    p.dma("pool", DMA(ODv[:, i * 4:(i + 1) * 4, hd * 128:(hd + 1) * 128], od_[:]), reads=[Bod_],
                          writes=[BOD], waw=False)

        load_head(head_list[0])
        emit_qk(0)
        for n in range(N):
            if n + 1 < N:
                emit_qk(n + 1)
            emit_exp(n)
            emit_av(n)
        p.barrier()
        p.emit()

    if stage < 4:
        return finish(nc, p, top, out_h, BOUT)

    with ExitStack() as es:
        wg8 = sbt(es, "wg8", [128, 8, 2048], BF16)
        wpm = sbt(es, "wpm", [128, 4, D], BF16)
        wpd = sbt(es, "wpd", [128, 4, D], BF16)
        wo = sbt(es, "wo", [128, 8, D], BF16)
        hTg = [sbt(es, "hTg%d" % i, [128, 8, 512], BF16) for i in range(2)]
        omt = [sbt(es, "omt%d" % i, [128, 4, 512], BF16) for i in range(2)]
        odt_ = [sbt(es, "odt%d" % i, [128, 4, 512], BF16) for i in range(2)]
        xg = [sbt(es, "xg%d" % i, [128, D], F32) for i in range(2)]
        omT = sbt(es, "omT", [128, 4, 512], BF16)
        odT = sbt(es, "odT", [128, 4, 512], BF16)
        sg = [sbt(es, "sg%d" % i, [128, 1024], F32) for i in range(2)]
        mT = sbt(es, "mT", [128, 8, 512], BF16)
        x1t = [sbt(es, "x1t%d" % i, [128, D], F32) for i in range(2)]
        sqj = sbt(es, "sqj", [128, D], BF16)
        ss = [sbt(es, "ss%d" % i, [128, 4], F32) for i in range(2)]
        h2 = [sbt(es, "h2%d" % i, [128, D], F32) for i in range(2)]
        h2b = [sbt(es, "h2b%d" % i, [128, D], BF16) for i in range(2)]
        h2T = sbt(es, "h2T", [128, 8, 128], F32)
        lg = sbt(es, "lg", [128, 36], F32)
        rs = sbt(es, "rs", [128, 32], F32)
        gsm = sbt(es, "gsm", [128, 16], F32)
        elm = sbt(es, "elm", [128, 32], F32)
        t8 = sbt(es, "t8", [128, 8], F32)
        oh1 = sbt(es, "oh1", [128, 32], F32)
        ohM = sbt(es, "ohM", [128, 32], F32)
        oh2 = sbt(es, "oh2", [128, 32], F32)
        ohb = sbt(es, "ohb", [128, 32], BF16)
        rank = sbt(es, "rank", [128, 32], F32)
        tq = sbt(es, "tq", [128, 32], F32)
        pT = pst(es, "pT", [128, 2048], BF16)
        pY = pst(es, "pY", [128, 1024], F32)
        pG = pst(es, "pG", [128, 1024], F32)
        pZ = pst(es, "pZ", [128, 1024], F32)
        (Bwg8, Bwpm, Bwpd, Bwo, BomT, BodT, Btm, BmT, Bsqj, Bh2T, Blg, Brs, Bgsm, Belm, Bt8, Boh1, BohM, Boh2, Bohb,
         Brank, Btq, BpT, BpY, BpG, BpZ, Bcarry, Bpos, Bwts) = [Buf(n) for n in (
            "wg8", "wpm", "wpd", "wo", "omT", "odT", "tm", "mT", "sqj", "h2T", "lg", "rs", "gsm", "elm", "t8", "oh1",
            "ohM", "oh2", "ohb", "rank", "tq", "pT", "pY", "pG", "pZ", "carry", "pos", "wts")]
        BhTg = [Buf("hTg%d" % i) for i in range(2)]
        Bomt = [Buf("omt%d" % i) for i in range(2)]
        Bodt_ = [Buf("odt%d" % i) for i in range(2)]
        Bxg = [Buf("xg%d" % i) for i in range(2)]
        Bsg = [Buf("sg%d" % i) for i in range(2)]
        Bx1t = [Buf("x1t%d" % i) for i in range(2)]
        Bss = [Buf("ss%d" % i) for i in range(2)]
        Bh2 = [Buf("h2%d" % i) for i in range(2)]
        Bh2b = [Buf("h2b%d" % i) for i in range(2)]

        for gi in range(4):
            p.dma("pool", DMA(wg8[:, :, gi * 512:(gi + 1) * 512],
                              w_in[:, 3072 + gi * 512:3072 + (gi + 1) * 512].rearrange("(k p) n -> p k n", p=128)),
                  writes=[Bwg8], waw=False)
        p.dma("pool", DMA(wpm[:], w_pm.rearrange("(k p) n -> p k n", p=128)), writes=[Bwpm])
        p.dma("pool", DMA(wpd[:], w_pd.rearrange("(k p) n -> p k n", p=128)), writes=[Bwpd])
        for gi in range(2):
            p.dma("pool", DMA(wo[:, :, gi * 512:(gi + 1) * 512], w_o[:, gi * 512:(gi + 1) * 512].rearrange("(k p) n -> p k n", p=128)),
                  writes=[Bwo], waw=False)
        for k in range(8):
            p.op("pool", TT(wo[:, k, :], wo[:, k, :], g1_bc[:], ALU.mult), reads=[Bwo], writes=[Bwo])
        OMr = OM.rearrange("(n p) c -> p n c", p=128)
        ODr = OD.rearrange("(n p) c -> p n c", p=128)
        XOr = xown.rearrange("(n p) c -> p n c", p=128)
        X1r = X1.rearrange("(n p) c -> p n c", p=128)

        def load_group(g):
            u = g % 2
            p.dma("sp", DMA(hTg[u][:], HT[:, :, g * 512:(g + 1) * 512]), reads=[BHT], writes=[BhTg[u]])
            p.dma("sp", DMA(omt[u][:], OMr[:, g * 4:(g + 1) * 4, :]), reads=[BOM], writes=[Bomt[u]])
            p.dma("sp", DMA(odt_[u][:], ODr[:, g * 4:(g + 1) * 4, :]), reads=[BOD], writes=[Bodt_[u]])

        h2b3 = h2b + [sbt(es, "h2b2", [128, D], BF16)]
        Bh2b3 = Bh2b + [Buf("h2b2")]
        lg2 = [lg, sbt(es, "lg1", [128, 36], F32)]
        Blg2 = [Blg, Buf("lg1")]
        pTv = pT[:].rearrange("p (c n) -> p c n", n=512)

        def g_pre(g):
            if g + 1 < 8:
                load_group(g + 1)
            u = g % 2
            p.op("pe", TRS([(pTv[:, cb, qs * 128:(qs + 1) * 128], omt[u][:, qs, cb * 128:(cb + 1) * 128], ident_b[:])
                            for qs in range(4) for cb in range(4)]), reads=[Bomt[u]], writes=[BpT])
            p.op("act", ACPY(omT[:], pTv), reads=[BpT], writes=[BomT])
            p.op("pe", TRS([(pTv[:, cb, qs * 128:(qs + 1) * 128], odt_[u][:, qs, cb * 128:(cb + 1) * 128], ident_b[:])
                            for qs in range(4) for cb in range(4)]), reads=[Bodt_[u]], writes=[BpT])
            p.op("dve", CPY(odT[:], pTv), reads=[BpT], writes=[BodT])
            for oc in range(8):
                osl = slice(oc * 128, (oc + 1) * 128)
                p.op("pe", MMS([(pG[:, 0:512], wg8[:, k, oc * 128:(oc + 1) * 128], hTg[u][:, k, :], k == 0, k == 7)
                                for k in range(8)] +
                               [(pG[:, 512:1024], wg8[:, k, 1024 + oc * 128:1024 + (oc + 1) * 128], hTg[u][:, k, :], k == 0, k == 7)
                                for k in range(8)]), reads=[Bwg8, BhTg[u]], writes=[BpG])
                p.op("pe", MMS([(pY[:, 0:512], wpm[:, kc, osl], omT[:, kc, :], kc == 0, kc == 3) for kc in range(4)] +
                               [(pY[:, 512:1024], wpd[:, kc, osl], odT[:, kc, :], kc == 0, kc == 3) for kc in range(4)]),
                     reads=[Bwpm, Bwpd, BomT, BodT], writes=[BpY])
                sgu, Bsgu = sg[oc % 2], Bsg[oc % 2]
                p.op("act", ACTF(sgu[:], pG[:], AF.Sigmoid), reads=[BpG], writes=[Bsgu])
                p.op("dve", TT(sgu[:], pY[:], sgu[:], ALU.mult), reads=[BpY], writes=[Bsgu])
                p.op("pool", TT(mT[:, oc, :], sgu[:, 0:512], sgu[:, 512:1024], ALU.add), reads=[Bsgu], writes=[BmT])

        def s0(t):
            g, qs = t // 4, t % 4
            if qs == 0:
                g_pre(g)
            v = t % 2
            p.dma("sp", DMA(xg[v][:], XOr[:, t, :]), writes=[Bxg[v]])
            p.op("pe", MMS([(pZ[:, nh * 512:(nh + 1) * 512], mT[:, k, qs * 128:(qs + 1) * 128], wo[:, k, nh * 512:(nh + 1) * 512],
                             k == 0, k == 7) for k in range(8) for nh in range(2)]), reads=[BmT, Bwo], writes=[BpZ])
            p.op("dve", TT(x1t[v][:], pZ[:], xg[v][:], ALU.add), reads=[BpZ, Bxg[v]], writes=[Bx1t[v]])
            p.dma("act", DMA(X1r[:, t, :], x1t[v][:]), reads=[Bx1t[v]], writes=[BX1], waw=False)
            p.op("act", ACTF(sqj[:], x1t[v][:], AF.Square, accum=ss[v][:, 0:1]), reads=[Bx1t[v]], writes=[Bsqj, Bss[v]])
            p.op("dve", TS(ss[v][:, 1:2], ss[v][:, 0:1], 1.0 / D, EPS, ALU.mult, ALU.add), reads=[Bss[v]], writes=[Bss[v]])
            p.op("pool", TT(ss[v][:, 2:3], ss[v][:, 1:2], nhalf[:, 0:1], ALU.pow), reads=[Bss[v]], writes=[Bss[v]])
            p.op("dve", STT(h2[v][:], x1t[v][:], ss[v][:, 2:3], a2_bc[:], ALU.mult, ALU.mult), reads=[Bx1t[v], Bss[v]],
                 writes=[Bh2[v]])
            p.op("dve", TT(h2[v][:], h2[v][:], b2_bc[:], ALU.add), reads=[Bh2[v]], writes=[Bh2[v]])
            p.op("act", ACPY(h2b3[t % 3][:], h2[v][:]), reads=[Bh2[v]], writes=[Bh2b3[t % 3]])

        def s1(t):
            v = t % 2
            lgv, Blgv = lg2[v], Blg2[v]
            p.op("pe", TRS([(pG[:, k * 128:(k + 1) * 128], h2[v][:, k * 128:(k + 1) * 128], ident_f[:]) for k in range(8)]),
                 reads=[Bh2[v]], writes=[BpG])
            p.op("act", ACPY(h2T[:], pG[:].rearrange("p (k n) -> p k n", n=128)), reads=[BpG], writes=[Bh2T])
            p.op("pe", MMS([(pY[:, 0:36], h2T[:, k, :], wr_sb[:, k, :], k == 0, k == 7) for k in range(8)]),
                 reads=[Bh2T], writes=[BpY])
            p.op("dve", TT(lgv[:], pY[:, 0:36], br_sb[:], ALU.add), reads=[BpY], writes=[Blgv])

        def s2(t):
            v = t % 2
            lg, Blg = lg2[v], Blg2[v]
            p.op("dve", RED(rs[:, 0:1], lg[:, 0:4], ALU.max), reads=[Blg], writes=[Brs])
            p.op("dve", TS(gsm[:, 0:4], lg[:, 0:4], rs[:, 0:1], None, ALU.subtract), reads=[Blg, Brs], writes=[Bgsm])
            p.op("act", ACTF(gsm[:, 4:8], gsm[:, 0:4], AF.Sigmoid), reads=[Bgsm], writes=[Bgsm])
            p.op("act", ACTF(gsm[:, 8:12], gsm[:, 0:4], AF.Sigmoid, scale=-1.0), reads=[Bgsm], writes=[Bgsm])
            p.op("dve", (lambda o, i_: (lambda e: e.reciprocal(out=o, in_=i_)))(gsm[:, 12:16], gsm[:, 8:12]),
                 reads=[Bgsm], writes=[Bgsm])
            p.op("dve", TT(gsm[:, 4:8], gsm[:, 4:8], gsm[:, 12:16], ALU.mult), reads=[Bgsm], writes=[Bgsm])
            p.op("dve", RED(rs[:, 1:2], gsm[:, 4:8], ALU.add), reads=[Bgsm], writes=[Brs])
            p.op("dve", (lambda o, i_: (lambda e: e.reciprocal(out=o, in_=i_)))(rs[:, 2:3], rs[:, 1:2]),
                 reads=[Brs], writes=[Brs])
            p.op("dve", TS(gsm[:, 0:4], lg[:, 0:4], rs[:, 0:1], None, ALU.is_ge), reads=[Blg, Brs], writes=[Bgsm])
            p.op("dve", TS(gsm[:, 0:4], gsm[:, 0:4], 1e30, -1e30, ALU.mult, ALU.add), reads=[Bgsm], writes=[Bgsm])
            p.op("dve", TT(elm[:].rearrange("p (g e) -> p g e", e=8), lg[:, 4:36].rearrange("p (g e) -> p g e", e=8),
                           bc(gsm[:, 0:4].unsqueeze(2), [128, 4, 8]), ALU.add), reads=[Blg, Bgsm], writes=[Belm])
            p.op("dve", (lambda o, i_: (lambda e: e.max(out=o, in_=i_)))(t8[:], elm[:]), reads=[Belm], writes=[Bt8])
            p.op("dve", TS(oh1[:], elm[:], t8[:, 0:1], None, ALU.is_equal), reads=[Belm, Bt8], writes=[Boh1])
            p.op("dve", TS(ohM[:], elm[:], t8[:, 1:2], None, ALU.is_ge), reads=[Belm, Bt8], writes=[BohM])
            p.op("dve", TT(oh2[:], ohM[:], oh1[:], ALU.subtract), reads=[BohM, Boh1], writes=[Boh2])
            p.op("dve", TT(rs[:, 3:4], t8[:, 0:1], t8[:, 1:2], ALU.subtract), reads=[Bt8], writes=[Brs])
            p.op("act", ACTF(rs[:, 4:5], rs[:, 3:4], AF.Sigmoid), reads=[Brs], writes=[Brs])
            p.op("dve", TS(rs[:, 5:6], rs[:, 4:5], -1.0, 1.0, ALU.mult, ALU.add), reads=[Brs], writes=[Brs])
            p.op("dve", TS(wts_sb[:, t, :], rs[:, 4:6], rs[:, 2:3], None, ALU.mult), reads=[Brs], writes=[Bwts])
            p.op("dve", CPY(ohb[:], ohM[:]), reads=[BohM], writes=[Bohb])
            p.op("pe", MMS([(pY[:, 64:96], utri_b[:], ohb[:], True, True), (pY[:, 96:128], ones_b[:], ohb[:], True, True)]),
                 reads=[Bohb], writes=[BpY])
            p.op("dve", TT(rank[:], pY[:, 64:96], carry[:], ALU.add), reads=[BpY, Bcarry], writes=[Brank])
            p.op("dve", TT(carry[:], carry[:], pY[:, 96:128], ALU.add), reads=[BpY, Brank], writes=[Bcarry])
            for j, oh in enumerate((oh1, oh2)):
                Boh = (Boh1, Boh2)[j]
                b0 = 8 + j * 8
                p.op("dve", TT(tq[:], oh[:], rank[:], ALU.mult), reads=[Boh, Brank], writes=[Btq])
                p.op("dve", RED(rs[:, b0:b0 + 1], tq[:], ALU.add), reads=[Btq], writes=[Brs])
                p.op("dve", TT(tq[:], oh[:], ebase_sb[:], ALU.mult), reads=[Boh, Brs], writes=[Btq])
                p.op("dve", RED(rs[:, b0 + 1:b0 + 2], tq[:], ALU.add), reads=[Btq], writes=[Brs])
                p.op("dve", TS(rs[:, b0 + 2:b0 + 3], rs[:, b0:b0 + 1], float(cap) - 0.5, 1e6, ALU.is_ge, ALU.mult),
                     reads=[Brs], writes=[Brs])
                p.op("dve", TT(rs[:, b0 + 3:b0 + 4], rs[:, b0:b0 + 1], rs[:, b0 + 1:b0 + 2], ALU.add), reads=[Brs], writes=[Brs])
                p.op("dve", TT(rs[:, b0 + 3:b0 + 4], rs[:, b0 + 3:b0 + 4], rs[:, b0 + 2:b0 + 3], ALU.add), reads=[Brs], writes=[Brs])
                p.op("dve", CPY(pos_i[:, t, j:j + 1], rs[:, b0 + 3:b0 + 4]), reads=[Brs], writes=[Bpos])
                p.dma("pool", (lambda idx, src: (lambda e: e.indirect_dma_start(
                    out=XS, out_offset=bass.IndirectOffsetOnAxis(ap=idx, axis=0), in_=src, in_offset=None,
                    bounds_check=bnd(e, "p3"), oob_is_err=False)))(pos_i[:, t, j:j + 1], h2b3[t % 3][:, :]),
                    reads=[Bpos, Bh2b3[t % 3]], writes=[BXS], waw=False)

        load_group(0)
        pipeline(NT_OWN, [s0, s1, s2])
        p.barrier()
        p.emit()

    if stage < 5:
        return finish(nc, p, top, out_h, BOUT)

    with ExitStack() as es:
        wgs = [sbt(es, "wgs%d" % i, [128, 8, 512], BF16) for i in range(2)]
        wus = [sbt(es, "wus%d" % i, [128, 8, 512], BF16) for i in range(2)]
        wds = [sbt(es, "wds%d" % i, [128, 4, D], BF16) for i in range(2)]
        xrow = [sbt(es, "xrow%d" % i, [128, 4, D], BF16) for i in range(2)]
        xsT = [sbt(es, "xsT%d" % i, [128, 8, 512], BF16) for i in range(2)]
        sgm = [sbt(es, "sgm%d" % i, [128, 512], F32) for i in range(2)]
        hidT = [sbt(es, "hidT%d" % i, [128, 4, 512], BF16) for i in range(2)]
        yrow = [sbt(es, "yrow%d" % i, [128, D], F32) for i in range(2)]
        pT = pst(es, "pT", [128, 2048], BF16)
        pGU = [pst(es, "pGU%d" % i, [128, 1024], F32) for i in range(2)]
        pOu = pst(es, "pOu", [128, 1024], F32)
        Bwgs = [Buf("wgs%d" % i) for i in range(2)]
        Bwus = [Buf("wus%d" % i) for i in range(2)]
        Bwds = [Buf("wds%d" % i) for i in range(2)]
        Bxrow = [Buf("xrow%d" % i) for i in range(2)]
        BxsT = [Buf("xsT%d" % i) for i in range(2)]
        Bsgm = [Buf("sgm%d" % i) for i in range(2)]
        BhidT = [Buf("hidT%d" % i) for i in range(2)]
        Byrow = [Buf("yrow%d" % i) for i in range(2)]
        BpT = Buf("pT")
        BpGU = [Buf("pGU%d" % i) for i in range(2)]
        BpOu = Buf("pOu")

        groups = []
        r0 = 0
        while r0 < cap:
            R = min(512, cap - r0)
            groups.append((r0, R))
            r0 += R

        def load_w(e):
            u = e % 2
            p.dma("pool", DMA(wgs[u][:], w_gate[e].rearrange("(k p) n -> p k n", p=128)), writes=[Bwgs[u]])
            p.dma("pool", DMA(wus[u][:], w_up[e].rearrange("(k p) n -> p k n", p=128)), writes=[Bwus[u]])
            p.dma("pool", DMA(wds[u][:], w_down[e].rearrange("(k p) n -> p k n", p=128)), writes=[Bwds[u]])

        xrow3 = xrow + [sbt(es, "xrow2", [128, 4, D], BF16)]
        Bxrow3 = Bxrow + [Buf("xrow2")]
        gitems = [(e, r0, R, gidx == 0) for e in range(32) for gidx, (r0, R) in enumerate(groups)]
        assert len(groups) >= 2
        pTv = pT[:].rearrange("p (k n) -> p k n", n=512)

        def e0(gi):
            e, r0, R, first = gitems[gi]
            nt = R // 128
            base = e * cap + r0
            p.dma("sp", DMA(xrow3[gi % 3][:, 0:nt, :], XS[base:base + R, :].rearrange("(n p) d -> p n d", p=128)),
                  reads=[BXS], writes=[Bxrow3[gi % 3]])

        def e1(gi):
            e, r0, R, first = gitems[gi]
            nt = R // 128
            v = gi % 2
            xr, Bxr = xrow3[gi % 3], Bxrow3[gi % 3]
            for half in range(2):
                p.op("pe", TRS([(pTv[:, kk, n * 128:(n + 1) * 128], xr[:, n, (half * 4 + kk) * 128:(half * 4 + kk + 1) * 128],
                                 ident_b[:]) for n in range(nt) for kk in range(4)]), reads=[Bxr], writes=[BpT])
                if half == 0:
                    p.op("act", ACPY(xsT[v][:, 0:4, 0:R], pTv[:, :, 0:R]), reads=[BpT], writes=[BxsT[v]])
                else:
                    p.op("dve", CPY(xsT[v][:, 4:8, 0:R], pTv[:, :, 0:R]), reads=[BpT], writes=[BxsT[v]])

        def e2(gi):
            e, r0, R, first = gitems[gi]
            u = e % 2
            v = gi % 2
            for hc in range(4):
                w_ = hc % 2
                hsl = slice(hc * 128, (hc + 1) * 128)
                p.op("pe", MMS([(pGU[w_][:, 0:R], wgs[u][:, k, hsl], xsT[v][:, k, 0:R], k == 0, k == 7) for k in range(8)] +
                               [(pGU[w_][:, 512:512 + R], wus[u][:, k, hsl], xsT[v][:, k, 0:R], k == 0, k == 7) for k in range(8)]),
                     reads=[Bwgs[u], Bwus[u], BxsT[v]], writes=[BpGU[w_]])
                p.op("act", ACTF(sgm[w_][:, 0:R], pGU[w_][:, 0:R], AF.Silu), reads=[BpGU[w_]], writes=[Bsgm[w_]])
                p.op("dve", TT(hidT[v][:, hc, 0:R], sgm[w_][:, 0:R], pGU[w_][:, 512:512 + R], ALU.mult),
                     reads=[Bsgm[w_], BpGU[w_]], writes=[BhidT[v]])

        ycount = [0]
        BpOuH = [Buf("pOu_h0"), Buf("pOu_h1")]
        ByrowH = [[Buf("yrow%d_h%d" % (i, h)) for h in range(2)] for i in range(2)]

        def e3(gi):
            e, r0, R, first = gitems[gi]
            u = e % 2
            v = gi % 2
            nt = R // 128
            base = e * cap + r0
            if first and e + 1 < 32:
                load_w(e + 1)
            for n in range(nt):
                y_ = ycount[0] % 2
                ycount[0] += 1
                for nh in range(2):
                    p.op("pe", MMS([(pOu[:, nh * 512:(nh + 1) * 512], hidT[v][:, hc, n * 128:(n + 1) * 128],
                                     wds[u][:, hc, nh * 512:(nh + 1) * 512], hc == 0, hc == 3) for hc in range(4)]),
                         reads=[BhidT[v], Bwds[u]], writes=[BpOuH[nh]])
                p.op("act", ACPY(yrow[y_][:, 0:512], pOu[:, 0:512]), reads=[BpOuH[0]], writes=[ByrowH[y_][0]])
                p.op("dve", CPY(yrow[y_][:, 512:1024], pOu[:, 512:1024]), reads=[BpOuH[1]], writes=[ByrowH[y_][1]])
                p.dma("sp", DMA(YS[base + n * 128:base + (n + 1) * 128, :], yrow[y_][:]), reads=[ByrowH[y_][0], ByrowH[y_][1]],
                      writes=[BYS], waw=False)

        load_w(0)
        pipeline(len(gitems), [e0, e1, e2, e3])
        p.barrier()
        p.emit()

    with ExitStack() as es:
        x1l = [sbt(es, "x1l%d" % i, [128, D], F32) for i in range(2)]
        r1 = [sbt(es, "r1%d" % i, [128, D], F32) for i in range(2)]
        r2 = [sbt(es, "r2%d" % i, [128, D], F32) for i in range(2)]
        yt = [sbt(es, "yt%d" % i, [128, D], F32) for i in range(2)]
        ot = [sbt(es, "ot%d" % i, [128, D], F32) for i in range(2)]
        sqj = sbt(es, "sqj", [128, D], BF16)
        ss = [sbt(es, "ss%d" % i, [128, 4], F32) for i in range(2)]
        Bx1l = [Buf("x1l%d" % i) for i in range(2)]
        Br1 = [Buf("r1%d" % i) for i in range(2)]
        Br2 = [Buf("r2%d" % i) for i in range(2)]
        Byt = [Buf("yt%d" % i) for i in range(2)]
        Bot = [Buf("ot%d" % i) for i in range(2)]
        Bss = [Buf("ss%d" % i) for i in range(2)]
        Bsqj = Buf("sqj")
        X1r = X1.rearrange("(n p) c -> p n c", p=128)
        OUTr = out_h.rearrange("(n p) c -> p n c", p=128)
        x1l3 = x1l + [sbt(es, "x1l2", [128, D], F32)]
        r13 = r1 + [sbt(es, "r12", [128, D], F32)]
        r23 = r2 + [sbt(es, "r22", [128, D], F32)]
        Bx1l3 = Bx1l + [Buf("x1l2")]
        Br13 = Br1 + [Buf("r12")]
        Br23 = Br2 + [Buf("r22")]

        def f0(t):
            w3 = t % 3
            p.dma("sp", DMA(x1l3[w3][:], X1r[:, t, :]), reads=[BX1], writes=[Bx1l3[w3]])
            for j, (rr, Brr) in enumerate(((r13[w3], Br13[w3]), (r23[w3], Br23[w3]))):
                p.op("pool", MSET(rr[:], 0.0), writes=[Brr])
                p.dma("pool", (lambda idx, dst: (lambda e: e.indirect_dma_start(
                    out=dst, out_offset=None, in_=YS, in_offset=bass.IndirectOffsetOnAxis(ap=idx, axis=0),
                    bounds_check=bnd(e, "p5"), oob_is_err=False)))(pos_i[:, t, j:j + 1], rr[:, :]),
                    reads=[BYS], writes=[Brr])

        def f1(t):
            v = t % 2
            w3 = t % 3
            p.op("dve", TS(yt[v][:], r13[w3][:], wts_sb[:, t, 0:1], None, ALU.mult), reads=[Br13[w3]], writes=[Byt[v]])
            p.op("dve", STT(yt[v][:], r23[w3][:], wts_sb[:, t, 1:2], yt[v][:], ALU.mult, ALU.add), reads=[Br23[w3]], writes=[Byt[v]])
            p.op("dve", TT(yt[v][:], yt[v][:], g2_bc[:], ALU.mult), reads=[Byt[v]], writes=[Byt[v]])
            p.op("dve", TT(yt[v][:], yt[v][:], x1l3[w3][:], ALU.add), reads=[Bx1l3[w3]], writes=[Byt[v]])
            p.op("act", ACTF(sqj[:], yt[v][:], AF.Square, accum=ss[v][:, 0:1]), reads=[Byt[v]], writes=[Bsqj, Bss[v]])
            p.op("dve", TS(ss[v][:, 1:2], ss[v][:, 0:1], 1.0 / D, EPS, ALU.mult, ALU.add), reads=[Bss[v]], writes=[Bss[v]])
            p.op("pool", TT(ss[v][:, 2:3], ss[v][:, 1:2], nhalf[:, 0:1], ALU.pow), reads=[Bss[v]], writes=[Bss[v]])
            p.op("dve", STT(ot[v][:], yt[v][:], ss[v][:, 2:3], fing_sb[:], ALU.mult, ALU.mult), reads=[Byt[v], Bss[v]], writes=[Bot[v]])
            p.dma("sp", DMA(OUTr[:, t, :], ot[v][:]), reads=[Bot[v]], writes=[BOUT], waw=False)

        pipeline(NT_OWN, [f0, f1])
        p.barrier()
        p.emit()

    return finish(nc, p, top, out_h, BOUT)


def finish(nc, p, top, out_h, BOUT):
    p.barrier()
    p.emit()
    p.close()
    top.close()
    return nc


def host_prep(inputs, cap=CAP):
    x = np.ascontiguousarray(inputs["x"], dtype=np.float32)
    c = inputs["c"]
    f32 = np.float32

    def rep(v, n=128):
        return np.ascontiguousarray(np.broadcast_to(np.asarray(v, dtype=f32).reshape(1, -1), (n, np.asarray(v).size)))

    inv = 1.0 / (10000.0 ** (np.arange(0, 64, 2, dtype=np.float32) / 64.0))
    ang = np.arange(S, dtype=np.float32)[:, None] * inv[None, :].astype(np.float32)
    cs = np.concatenate([np.cos(ang), np.sin(ang)], axis=1).astype(f32)
    khot = np.zeros((33, S), f32)
    for n in range(32):
        khot[n, n * 256:(n + 1) * 256] = 1.0
    khot[32, :] = 1.0
    kk = np.arange(128)[:, None]
    qq = np.arange(512)[None, :]
    diag = [((j * 128 + kk) <= qq).astype(f32) for j in range(4)]
    ones = np.ones((128, 512), f32)
    zeros = np.zeros((128, 512), f32)
    patA = diag + [zeros] * 4
    patB = [ones] * 4 + diag
    ident = np.eye(128, dtype=f32)
    utri = (np.arange(128)[:, None] < np.arange(128)[None, :]).astype(f32)
    lam4 = np.stack([rep(inputs[k][0]) for k in ("lambda_q1", "lambda_k1", "lambda_q2", "lambda_k2")], axis=1)
    shared = dict(
        w_ada=np.ascontiguousarray(inputs["w_ada"][0]), bada_bc=rep(inputs["b_ada"][0]),
        n1g_bc=rep(inputs["norm1_g"][0]), n2g_bc=rep(inputs["norm2_g"][0]), fing_bc=rep(inputs["final_g"]),
        w_in=np.ascontiguousarray(inputs["w_in"][0]), khot=khot, ident=ident, utri=utri,
        lam4=np.ascontiguousarray(lam4), subg_bc=rep(inputs["diff_subln_g"][0]),
        w_pm=np.ascontiguousarray(inputs["w_proj_moba"][0]), w_pd=np.ascontiguousarray(inputs["w_proj_diff"][0]),
        w_o=np.ascontiguousarray(inputs["w_out"][0]),
        w_r=np.ascontiguousarray(np.concatenate([inputs["w_group"][0], inputs["w_expert"][0]], axis=1)),
        br_bc=rep(np.concatenate([inputs["b_group"][0], inputs["b_expert"][0]])),
        ebase_bc=rep(np.arange(32, dtype=f32) * cap),
        w_gate=np.ascontiguousarray(inputs["w_gate"][0]), w_up=np.ascontiguousarray(inputs["w_up"][0]),
        w_down=np.ascontiguousarray(inputs["w_down"][0]),
        cs_all=np.ascontiguousarray(cs.reshape(NT_ALL, 128, 64).transpose(1, 0, 2)),
    )
    in_maps, own_idx = [], []
    for core in range(8):
        b, pp = core // 2, core % 2
        chunks = OWN_CHUNKS[pp]
        idx = np.concatenate([np.arange(ch * 512, (ch + 1) * 512) for ch in chunks])
        own_idx.append((b, idx))
        pats = []
        for par in range(2):
            cch = chunks[par]
            pats.append(patA if cch % 2 == 0 else patB)
        cmask = np.stack(pats[0] + pats[1], axis=0)
        blk = idx // 256
        nn = np.arange(32)[None, :]
        bval = np.where(nn < blk[:, None], 0.0, -1e30).astype(f32)
        oblk = (nn == blk[:, None]).astype(f32)
        m = dict(shared)
        m.update(
            xall=x[b], xown=np.ascontiguousarray(x[b][idx]),
            c_col=np.ascontiguousarray(c[b].reshape(8, 128).T.astype(f32)),
            cs_own=np.ascontiguousarray(cs[idx].reshape(NT_OWN, 128, 64).transpose(1, 0, 2)),
            cmask=np.ascontiguousarray(cmask.transpose(1, 0, 2)),
            blkvalid=np.ascontiguousarray(bval.reshape(NT_OWN, 128, 32).transpose(1, 0, 2)),
            ownblk=np.ascontiguousarray(oblk.reshape(NT_OWN, 128, 32).transpose(1, 0, 2)),
        )
        in_maps.append(m)
    return in_maps, own_idx


def kernel(**inputs):
    in_maps, own_idx = host_prep(inputs)
    nc = build()
    res = run_bass_kernel_spmd(nc, in_maps, core_ids=list(range(8)))
    out = np.empty((4, S, D), np.float32)
    for core in range(8):
        b, idx = own_idx[core]
        out[b, idx, :] = res.results[core]["out"]
    return out
```
